# Optimizing a Trainium2 kernel written in Bass

```python
import jax, jax.numpy as jnp
from jax import lax
import numpy as np

D_MODEL = 2048
BATCH = 4
SEQ = 2048
DEPTH = 4

GRID_W = 64
CTX_LEN = 256
N_MIXERS = 2
EPS = 1e-6
ROPE_BASE = 10000.0
GLA_HEADS = 4
GLA_QK_DIM = D_MODEL // 2
GLA_DK = GLA_QK_DIM // GLA_HEADS
GLA_DV = D_MODEL // GLA_HEADS
GLA_GATE_RANK = 16
GLA_TAU = 16.0
GLA_CHUNK = 64
GLA_SPLITS = (GLA_QK_DIM, 2 * GLA_QK_DIM, 2 * GLA_QK_DIM + D_MODEL, 2 * GLA_QK_DIM + 2 * D_MODEL, 2 * GLA_QK_DIM + 2 * D_MODEL + GLA_GATE_RANK)
GLA_IN_COLS = 2 * GLA_QK_DIM + 2 * D_MODEL + 2 * GLA_GATE_RANK
NAT_HEADS = 16
NAT_DH = D_MODEL // NAT_HEADS
WIN_R = 8
WIN_C = 16
Q_BLOCK_C = 16
KV_BLOCK_C = 32
D_FF = 5632
N_EXPERTS = 8
TOP_K = 2
D_FF_EXPERT = 5632

kernel_name = 'hybrid_gla_natten_moe_dit'


def rmsnorm(x, g):
    x32 = x.astype(jnp.float32)
    y = x32 * lax.rsqrt(jnp.mean(x32 * x32, axis=-1, keepdims=True) + EPS)
    return (y * g.astype(jnp.float32)).astype(x.dtype)


def modulate(x, g, shift, scale):
    return rmsnorm(x, g) * (1 + scale) + shift


def ada_modulation(cvec, w, b):
    m = jax.nn.silu(cvec) @ w + b
    m = m.reshape(-1, 1, m.shape[-1])
    return jnp.split(m, 6, axis=-1)


def split_heads(a, n_heads):
    b, n, d = a.shape
    return a.reshape(b, n, n_heads, d // n_heads).transpose(0, 2, 1, 3)


def merge_heads(a):
    b, h, n, d = a.shape
    return a.transpose(0, 2, 1, 3).reshape(b, n, h * d)


def axial_rope(x):
    n, dim = x.shape[-2], x.shape[-1]
    half = dim // 2
    freqs = ROPE_BASE ** (-jnp.arange(0, half, 2, dtype=jnp.float32) / half)
    t = jnp.arange(n, dtype=jnp.int32)
    pos = jnp.stack([t // GRID_W, t % GRID_W], axis=-1).astype(jnp.float32)
    ang = pos[:, :, None] * freqs
    cos, sin = jnp.cos(ang), jnp.sin(ang)
    xs = x.astype(jnp.float32).reshape(x.shape[:-1] + (2, half))
    x1, x2 = xs[..., : half // 2], xs[..., half // 2:]
    rot = jnp.concatenate([x1 * cos - x2 * sin, x1 * sin + x2 * cos], axis=-1)
    return rot.reshape(x.shape).astype(x.dtype)


def gla_chunked(q, k, v, g, s0):
    b_, h_, n, _ = q.shape
    nc = n // GLA_CHUNK

    def to_chunks(a):
        return jnp.moveaxis(a.reshape(b_, h_, nc, GLA_CHUNK, a.shape[-1]), 2, 0)

    causal = jnp.tril(jnp.ones((GLA_CHUNK, GLA_CHUNK), dtype=bool))

    def step(s, inp):
        qc, kc, vc, gc = inp
        bcum = jnp.cumsum(gc, axis=2)
        o_inter = jnp.einsum('bhik,bhkv->bhiv', qc * jnp.exp(bcum), s)
        diff = bcum[:, :, :, None, :] - bcum[:, :, None, :, :]
        decay = jnp.exp(jnp.where(causal[:, :, None], diff, -jnp.inf))
        a = jnp.einsum('bhik,bhjk,bhijk->bhij', qc, kc, decay)
        o = o_inter + jnp.einsum('bhij,bhjv->bhiv', a, vc)
        b_last = bcum[:, :, -1:, :]
        s_new = jnp.exp(b_last[:, :, 0, :, None]) * s + jnp.einsum('bhjk,bhjv->bhkv', kc * jnp.exp(b_last - bcum), vc)
        return s_new, o

    s_fin, o = lax.scan(step, s0, (to_chunks(q), to_chunks(k), to_chunks(v), to_chunks(g)))
    o = jnp.moveaxis(o, 0, 2).reshape(b_, h_, n, v.shape[-1])
    return o, s_fin


def gla_final_state(k, v, g):
    bcum = jnp.cumsum(g, axis=2)
    return jnp.einsum('bhjk,bhjv->bhkv', k * jnp.exp(bcum[:, :, -1:] - bcum), v)


def gla_mixer(h, hc, w_in, wg_f, bg_f, wg_b, bg_b, norm_g, w_out, need_ctx_out):
    def project(a, rotary):
        q, k, v, r, zf, zb = jnp.split(a @ w_in, GLA_SPLITS, axis=-1)
        q = split_heads(q, GLA_HEADS) * GLA_DK ** -0.5
        k = split_heads(k, GLA_HEADS)
        if rotary:
            q, k = axial_rope(q), axial_rope(k)
        v = split_heads(v, GLA_HEADS)
        gf = split_heads(jax.nn.log_sigmoid((zf @ wg_f + bg_f).astype(jnp.float32)) / GLA_TAU, GLA_HEADS)
        gb = split_heads(jax.nn.log_sigmoid((zb @ wg_b + bg_b).astype(jnp.float32)) / GLA_TAU, GLA_HEADS)
        return q, k, v, r, gf, gb

    def readout(o, r):
        o = merge_heads(rmsnorm(o, norm_g))
        return ((o * jax.nn.silu(r)) @ w_out).astype(h.dtype)

    def rev(a):
        return jnp.flip(a, axis=2)

    q, k, v, r, gf, gb = project(h, True)
    qc, kc, vc, rc, gfc, gbc = project(hc, False)
    if need_ctx_out:
        zeros = jnp.zeros((h.shape[0], GLA_HEADS, GLA_DK, GLA_DV), jnp.float32)
        oc_f, s_f = gla_chunked(qc, kc, vc, gfc, zeros)
        oc_b, s_b = gla_chunked(rev(qc), rev(kc), rev(vc), rev(gbc), zeros)
        y_ctx = readout(oc_f + rev(oc_b), rc)
    else:
        s_f = gla_final_state(kc, vc, gfc)
        s_b = gla_final_state(rev(kc), rev(vc), rev(gbc))
        y_ctx = None
    o_f, _ = gla_chunked(q, k, v, gf, s_f)
    o_b, _ = gla_chunked(rev(q), rev(k), rev(v), rev(gb), s_b)
    return readout(o_f + rev(o_b), r), y_ctx


def nat_mixer(h, hc, w_in, rpb, w_out, need_ctx_out):
    b_, s_, _ = h.shape
    rows = s_ // GRID_W
    kr = min(WIN_R, rows)
    q, k, v = [split_heads(a, NAT_HEADS) for a in jnp.split(h @ w_in, 3, axis=-1)]
    qc, kc, vc = [split_heads(a, NAT_HEADS) for a in jnp.split(hc @ w_in, 3, axis=-1)]
    scale = NAT_DH ** -0.5
    qg = q.reshape(b_, NAT_HEADS, rows, GRID_W, NAT_DH) * scale
    kg = k.reshape(b_, NAT_HEADS, rows, GRID_W, NAT_DH)
    vg = v.reshape(b_, NAT_HEADS, rows, GRID_W, NAT_DH)
    n_cb = GRID_W // Q_BLOCK_C
    cb_start = np.clip(np.arange(n_cb) * Q_BLOCK_C - (KV_BLOCK_C - Q_BLOCK_C) // 2, 0, GRID_W - KV_BLOCK_C)
    key_cols = cb_start[:, None] + np.arange(KV_BLOCK_C)
    q_cols = np.arange(GRID_W).reshape(n_cb, Q_BLOCK_C)
    col_start = np.clip(q_cols - WIN_C // 2, 0, GRID_W - WIN_C)
    kcol = key_cols[:, None, :]
    in_win = (kcol >= col_start[..., None]) & (kcol < col_start[..., None] + WIN_C)
    dc_idx = np.clip(kcol - q_cols[..., None] + WIN_C - 1, 0, 2 * WIN_C - 2)
    n_lat = kr * KV_BLOCK_C

    def row_block(r):
        rs = jnp.clip(r - kr // 2, 0, rows - kr)
        k_blk = lax.dynamic_slice_in_dim(kg, rs, kr, axis=2)[:, :, :, key_cols]
        v_blk = lax.dynamic_slice_in_dim(vg, rs, kr, axis=2)[:, :, :, key_cols]
        q_blk = lax.dynamic_index_in_dim(qg, r, axis=2, keepdims=False).reshape(b_, NAT_HEADS, n_cb, Q_BLOCK_C, NAT_DH)
        s_lat = jnp.einsum('bhjqd,bhrjcd->bhjqrc', q_blk, k_blk).astype(jnp.float32)
        dr_idx = rs + jnp.arange(kr) - r + (WIN_R - 1)
        bias = jnp.transpose(rpb[:, dr_idx][:, :, dc_idx], (0, 2, 3, 1, 4))
        s_lat = jnp.where(in_win[:, :, None, :], s_lat + bias, -jnp.inf)
        s_ctx = jnp.einsum('bhjqd,bhld->bhjql', q_blk, kc).astype(jnp.float32)
        s_all = jnp.concatenate([s_lat.reshape(b_, NAT_HEADS, n_cb, Q_BLOCK_C, n_lat), s_ctx], axis=-1)
        p = jax.nn.softmax(s_all, axis=-1).astype(v.dtype)
        p_lat = p[..., :n_lat].reshape(b_, NAT_HEADS, n_cb, Q_BLOCK_C, kr, KV_BLOCK_C)
        o = jnp.einsum('bhjqrc,bhrjcd->bhjqd', p_lat, v_blk) + jnp.einsum('bhjql,bhld->bhjqd', p[..., n_lat:], vc)
        return o.reshape(b_, NAT_HEADS, GRID_W, NAT_DH)

    o = lax.map(row_block, jnp.arange(rows))
    o = jnp.transpose(o, (1, 0, 3, 2, 4)).reshape(b_, s_, D_MODEL)
    y = (o @ w_out).astype(h.dtype)
    y_ctx = None
    if need_ctx_out:
        sc = jnp.einsum('bhqd,bhld->bhql', qc * scale, kc).astype(jnp.float32)
        oc = jnp.einsum('bhql,bhld->bhqd', jax.nn.softmax(sc, axis=-1).astype(vc.dtype), vc)
        y_ctx = (merge_heads(oc) @ w_out).astype(hc.dtype)
    return y, y_ctx


def swiglu(t, wg, wu, wd):
    return (jax.nn.silu(t @ wg) * (t @ wu)) @ wd


def moe_swiglu(h, router, wg, wu, wd):
    shp = h.shape
    t = h.reshape(-1, shp[-1])
    logits = (t @ router).astype(jnp.float32)
    top_v, top_i = lax.top_k(logits, TOP_K)
    w = jax.nn.softmax(top_v, axis=-1)
    gates = jnp.sum(jax.nn.one_hot(top_i, N_EXPERTS, dtype=jnp.float32) * w[..., None], axis=1)
    out = jnp.zeros_like(t)
    for e in range(N_EXPERTS):
        out = out + gates[:, e:e + 1].astype(t.dtype) * swiglu(t, wg[e], wu[e], wd[e])
    return out.reshape(shp)


def setup_inputs(seed: int = 0) -> dict:
    key = jax.random.key(seed)
    ks = iter(jax.random.split(key, 32))
    d = D_MODEL
    n_even = (DEPTH + 1) // 2
    n_odd = DEPTH // 2

    def nrm(shape, scale):
        return jax.random.normal(next(ks), shape, jnp.float32) * scale

    return {
        'x': nrm((BATCH, SEQ, d), 1.0),
        'c': nrm((BATCH, d), 1.0),
        'ctx': nrm((BATCH, CTX_LEN, d), 1.0),
        'c_ctx': nrm((d,), 1.0),
        'ada_w': nrm((DEPTH, d, 6 * d), 0.5 * d ** -0.5),
        'ada_b': nrm((DEPTH, 6 * d), 0.02),
        'norm_mix_g': 1.0 + nrm((DEPTH, d), 0.02),
        'norm_ffn_g': 1.0 + nrm((DEPTH, d), 0.02),
        'gla_w_in': nrm((n_even, d, GLA_IN_COLS), d ** -0.5),
        'gla_wg_fwd': nrm((n_even, GLA_GATE_RANK, GLA_QK_DIM), GLA_GATE_RANK ** -0.5),
        'gla_bg_fwd': nrm((n_even, GLA_QK_DIM), 0.1),
        'gla_wg_bwd': nrm((n_even, GLA_GATE_RANK, GLA_QK_DIM), GLA_GATE_RANK ** -0.5),
        'gla_bg_bwd': nrm((n_even, GLA_QK_DIM), 0.1),
        'gla_norm_g': 1.0 + nrm((n_even, GLA_DV), 0.02),
        'gla_w_out': nrm((n_even, d, d), d ** -0.5),
        'nat_w_in': nrm((n_odd, d, 3 * d), d ** -0.5),
        'nat_rpb': nrm((n_odd, NAT_HEADS, 2 * WIN_R - 1, 2 * WIN_C - 1), 0.1),
        'nat_w_out': nrm((n_odd, d, d), d ** -0.5),
        'ffn_w_gate': nrm((n_even, d, D_FF), d ** -0.5),
        'ffn_w_up': nrm((n_even, d, D_FF), d ** -0.5),
        'ffn_w_down': nrm((n_even, D_FF, d), D_FF ** -0.5),
        'moe_router': nrm((n_odd, d, N_EXPERTS), d ** -0.5),
        'moe_w_gate': nrm((n_odd, N_EXPERTS, d, D_FF_EXPERT), d ** -0.5),
        'moe_w_up': nrm((n_odd, N_EXPERTS, d, D_FF_EXPERT), d ** -0.5),
        'moe_w_down': nrm((n_odd, N_EXPERTS, D_FF_EXPERT, d), D_FF_EXPERT ** -0.5),
        'final_norm_g': 1.0 + nrm((d,), 0.02),
    }


def reference(x, c, ctx, c_ctx, ada_w, ada_b, norm_mix_g, norm_ffn_g, gla_w_in, gla_wg_fwd, gla_bg_fwd, gla_wg_bwd, gla_bg_bwd, gla_norm_g, gla_w_out, nat_w_in, nat_rpb, nat_w_out, ffn_w_gate, ffn_w_up, ffn_w_down, moe_router, moe_w_gate, moe_w_up, moe_w_down, final_norm_g):
    seq = x.shape[1]
    xc = ctx
    for i in range(DEPTH):
        j = i // N_MIXERS
        last = i == DEPTH - 1
        sh1, sc1, g1, sh2, sc2, g2 = ada_modulation(c, ada_w[i], ada_b[i])
        csh1, csc1, cg1, csh2, csc2, cg2 = ada_modulation(c_ctx, ada_w[i], ada_b[i])
        h = modulate(x, norm_mix_g[i], sh1, sc1)
        hc = modulate(xc, norm_mix_g[i], csh1, csc1)
        if i % N_MIXERS == 0:
            y, yc = gla_mixer(h, hc, gla_w_in[j], gla_wg_fwd[j], gla_bg_fwd[j], gla_wg_bwd[j], gla_bg_bwd[j], gla_norm_g[j], gla_w_out[j], not last)
        else:
            y, yc = nat_mixer(h, hc, nat_w_in[j], nat_rpb[j], nat_w_out[j], not last)
        x = x + g1 * y
        h2 = modulate(x, norm_ffn_g[i], sh2, sc2)
        if not last:
            xc = xc + cg1 * yc
            h2 = jnp.concatenate([modulate(xc, norm_ffn_g[i], csh2, csc2), h2], axis=1)
        if i % 2 == 0:
            f = swiglu(h2, ffn_w_gate[j], ffn_w_up[j], ffn_w_down[j])
        else:
            f = moe_swiglu(h2, moe_router[j], moe_w_gate[j], moe_w_up[j], moe_w_down[j])
        x = x + g2 * f[:, -seq:]
        if not last:
            xc = xc + cg2 * f[:, : xc.shape[1]]
    return rmsnorm(x, final_norm_g)
```

```python
import contextlib
import numpy as np
import ml_dtypes
import concourse.bass as bass
import concourse.mybir as mybir
from concourse.bass_utils import run_bass_kernel_spmd

F32 = mybir.dt.float32; BF16 = mybir.dt.bfloat16
AF = mybir.ActivationFunctionType; ALU = mybir.AluOpType; AX = mybir.AxisListType
D = 2048; KC = 16; EPS = 1e-6
T = 1152; H = 384; NE = 8
SB = 2304; NTB = 18; DH = 128; DV = 512
NL = 4; NCOL = 1536; NR = 5


ENGS = ("tensor", "vector", "scalar", "gpsimd", "sync")


class Prog:
    def __init__(self, nc, n_dma_sems=24):
        self.nc = nc
        self.ops = []
        self.cnt = {e: 0 for e in ENGS}
        self.last_w = {}
        self.readers = {}
        self.waited = {e: {} for e in ENGS}
        self.n_dma_sems = n_dma_sems
        self.dma_cnt = [0] * n_dma_sems
        self.dma_busy = [None] * n_dma_sems

    def _deps(self, eng, reads, writes):
        need = []
        for t in reads:
            if t in self.last_w:
                need.append(self.last_w[t])
        for t in writes:
            if t in self.last_w:
                need.append(self.last_w[t])
            need.extend(self.readers.get(t, ()))
        out = {}
        for key, c in need:
            if c > self.waited[eng].get(key, 0) and c > out.get(key, 0):
                out[key] = c
        for key, c in out.items():
            self.waited[eng][key] = c
        return sorted(out.items(), key=lambda kv: str(kv[0]))

    def _commit(self, comp, reads, writes):
        for t in reads:
            self.readers.setdefault(t, []).append(comp)
        for t in writes:
            self.last_w[t] = comp
            self.readers[t] = []

    def op(self, eng, emit, reads=(), writes=()):
        waits = self._deps(eng, reads, writes)
        self.cnt[eng] += 1
        comp = (("e", eng), self.cnt[eng])
        self.ops.append(dict(eng=eng, emit=emit, waits=waits, inc=comp))
        self._commit(comp, reads, writes)
        return comp

    def dma(self, eng, emit, reads=(), writes=(), slot=None):
        if slot is None:
            slot = min(range(self.n_dma_sems), key=lambda i: self.dma_cnt[i])
        waits = dict(self._deps(eng, reads, writes))
        prev = self.dma_busy[slot]
        if prev is not None and prev[1] > self.waited[eng].get(prev[0], 0):
            waits[prev[0]] = max(waits.get(prev[0], 0), prev[1])
            self.waited[eng][prev[0]] = prev[1]
        self.dma_cnt[slot] += 1
        comp = (("d", slot), 16 * self.dma_cnt[slot])
        self.dma_busy[slot] = comp
        self.ops.append(dict(eng=eng, emit=emit, waits=sorted(waits.items(), key=lambda kv: str(kv[0])), inc=comp, dma=True))
        self._commit(comp, reads, writes)
        return comp

    def e(self, eng, name, r=(), w=(), **kw):
        return self.op(eng, lambda en, name=name, kw=kw: getattr(en, name)(**kw), reads=r, writes=w)

    def mm(self, out, lhsT, rhs, start, stop, r=(), w=()):
        return self.op("tensor", lambda en, a=(out, lhsT, rhs), s=(start, stop): en.matmul(a[0], a[1], a[2], start=s[0], stop=s[1]), reads=r, writes=w)

    def d(self, eng, out, in_, r=(), w=(), slot=None):
        return self.dma(eng, lambda en, a=(out, in_): en.dma_start(out=a[0], in_=a[1]), reads=r, writes=w, slot=slot)

    def final_wait(self, eng, comps):
        w = {}
        for key, c in comps:
            w[key] = max(w.get(key, 0), c)
        self.ops.append(dict(eng=eng, emit=None, waits=sorted(w.items(), key=lambda kv: str(kv[0])), inc=None))

    def run(self):
        nc = self.nc
        with contextlib.ExitStack() as st:
            esem = {e: st.enter_context(nc.semaphore("se_" + e)) for e in ENGS}
            dsem = [st.enter_context(nc.semaphore("sd_%d" % i)) for i in range(self.n_dma_sems)]
            block = st.enter_context(nc.Block())

            def sem_of(key):
                return esem[key[1]] if key[0] == "e" else dsem[key[1]]

            def replay(engname):
                def body(e):
                    for o in self.ops:
                        if o["eng"] != engname:
                            continue
                        for key, c in o["waits"]:
                            e.wait_ge(sem_of(key), c)
                        if o["emit"] is None:
                            continue
                        ins = o["emit"](e)
                        key, c = o["inc"]
                        ins.then_inc(sem_of(key), 16 if key[0] == "d" else 1)
                return body

            block.tensor(replay("tensor"))
            block.vector(replay("vector"))
            block.scalar(replay("scalar"))
            block.gpsimd(replay("gpsimd"))
            block.sync(replay("sync"))


def build_ffn(NTOK, NFC, TT=768):
    assert TT == 768 and NTOK % TT == 0
    NT = NTOK // TT; H = TT // 2
    nc = bass.Bass("TRN2", target_bir_lowering=False)
    hT = nc.dram_tensor("hT", [D, NTOK], BF16, kind="ExternalInput").ap()
    wg = nc.dram_tensor("wg", [NFC, 128, KC * 128], F32, kind="ExternalInput").ap()
    wu = nc.dram_tensor("wu", [NFC, 128, KC * 128], F32, kind="ExternalInput").ap()
    wd = nc.dram_tensor("wd", [KC, 128, NFC * 128], F32, kind="ExternalInput").ap()
    gate = nc.dram_tensor("gate", [1, NTOK], F32, kind="ExternalInput").ap()
    yT = nc.dram_tensor("yT", [D, NTOK], BF16, kind="ExternalOutput").ap()
    hv = hT.rearrange("(c p) t -> p c t", p=128)
    yv = yT.rearrange("(c p) t -> p c t", p=128)
    with contextlib.ExitStack() as st:
        sb = lambda n, s, d: st.enter_context(nc.sbuf_tensor(n, s, d))
        hs = [sb("hs%d" % i, [128, KC, TT], BF16) for i in range(2)]
        act = sb("act", [128, NFC, TT], BF16)
        wgs = [sb("wgs%d" % i, [128, KC * 128], BF16) for i in range(2)]
        wus = [sb("wus%d" % i, [128, KC * 128], BF16) for i in range(2)]
        wds = [sb("wds%d" % i, [128, NFC * 128], BF16) for i in range(2)]
        g1 = sb("g1", [1, TT], F32); ones = sb("ones", [1, 128], F32)
        gbc = sb("gbc", [128, TT], F32)
        sg = [sb("sg%d" % i, [128, TT], F32) for i in range(2)]
        yo = [sb("yo%d" % i, [128, TT], BF16) for i in range(2)]
        ps = st.enter_context(nc.psum_tensor("ps", [128, 8, 512], F32))
        P = Prog(nc)
        P.e("vector", "memset", w=["ones"], ap=ones[:], constant=1.0)
        outs = []
        def load_h(tt):
            P.d("sync", hs[tt % 2][:], hv[:, :, tt * TT:(tt + 1) * TT], w=[("hs", tt % 2)])
        def load_w1(fc, b):
            P.d("gpsimd", wgs[b][:], wg[fc], w=[("wg", b)])
            P.d("gpsimd", wus[b][:], wu[fc], w=[("wu", b)])
        def load_w2(oc, b):
            P.d("gpsimd", wds[b][:], wd[oc], w=[("wd", b)])
        w1n = 0; w2n = 0
        load_h(0)
        load_w1(0, 0)
        for tt in range(NT):
            hb = tt % 2
            if tt + 1 < NT:
                load_h(tt + 1)
            P.d("sync", g1[:], gate[:, tt * TT:(tt + 1) * TT], w=["g1"])
            for hf in range(2):
                P.mm(ps[:, hf, 0:H], ones[:], g1[:, hf * H:(hf + 1) * H], True, True, r=["ones", "g1"], w=[("ps", hf)])
                P.e("vector", "tensor_copy", r=[("ps", hf)], w=["gbc"], out=gbc[:, hf * H:(hf + 1) * H], in_=ps[:, hf, 0:H])
            for fc in range(NFC):
                wb = w1n % 2; w1n += 1
                if fc + 1 < NFC:
                    load_w1(fc + 1, w1n % 2)
                else:
                    load_w2(0, w2n % 2)
                s = fc % 2
                for (wt, tok, base) in ((wgs, "wg", 4 * s), (wus, "wu", 4 * s + 2)):
                    for kc in range(KC):
                        for hf in range(2):
                            P.mm(ps[:, base + hf, 0:H], wt[wb][:, kc * 128:(kc + 1) * 128], hs[hb][:, kc, hf * H:(hf + 1) * H],
                                 kc == 0, kc == KC - 1, r=[(tok, wb), ("hs", hb)], w=[("ps", base + hf)])
                for hf in range(2):
                    P.e("scalar", "activation", r=[("ps", 4 * s + hf)], w=[("sg", s, hf)],
                        out=sg[s][:, hf * H:(hf + 1) * H], in_=ps[:, 4 * s + hf, 0:H], func=AF.Silu)
                    P.e("vector", "tensor_tensor", r=[("sg", s, hf), ("ps", 4 * s + 2 + hf)], w=[("act", fc)],
                        out=act[:, fc, hf * H:(hf + 1) * H], in0=sg[s][:, hf * H:(hf + 1) * H], in1=ps[:, 4 * s + 2 + hf, 0:H], op=ALU.mult)
            for oc in range(KC):
                wb = w2n % 2; w2n += 1
                if oc + 1 < KC:
                    load_w2(oc + 1, w2n % 2)
                elif tt + 1 < NT:
                    load_w1(0, w1n % 2)
                pb = 2 * (oc % 4)
                for fc in range(NFC):
                    for hf in range(2):
                        P.mm(ps[:, pb + hf, 0:H], wds[wb][:, fc * 128:(fc + 1) * 128], act[:, fc, hf * H:(hf + 1) * H],
                             fc == 0, fc == NFC - 1, r=[("wd", wb), ("act", fc)], w=[("ps", pb + hf)])
                ob = oc % 2
                for hf in range(2):
                    P.e("vector", "tensor_tensor", r=[("ps", pb + hf), "gbc"], w=[("yo", ob)],
                        out=yo[ob][:, hf * H:(hf + 1) * H], in0=ps[:, pb + hf, 0:H], in1=gbc[:, hf * H:(hf + 1) * H], op=ALU.mult)
                outs.append(P.d("sync", yv[:, oc, tt * TT:(tt + 1) * TT], yo[ob][:], r=[("yo", ob)], w=[("out", tt, oc)]))
        P.final_wait("sync", outs)
        P.run()
    return nc

def lay_w1(w, NFC):
    FF = w.shape[1]
    if FF < NFC * 128:
        w = np.concatenate([w, np.zeros((w.shape[0], NFC * 128 - FF), w.dtype)], 1)
    return np.ascontiguousarray(w.reshape(KC, 128, NFC, 128).transpose(2, 1, 0, 3)).reshape(NFC, 128, KC * 128)

def lay_w2(w, NFC):
    FF = w.shape[0]
    if FF < NFC * 128:
        w = np.concatenate([w, np.zeros((NFC * 128 - FF, w.shape[1]), w.dtype)], 0)
    return np.ascontiguousarray(w.reshape(NFC, 128, KC, 128).transpose(2, 1, 0, 3)).reshape(KC, 128, NFC * 128)


SEGS = ((0, 256), (256, 1152))
PIECES = ((0, 0, 256, 0), (0, 256, 384, 1), (1, 0, 384, 1), (2, 0, 384, 1))

def build_tok(kind, moe=False, final=False):
    nc = bass.Bass("TRN2", target_bir_lowering=False)
    NV = 5 if kind == "T0" else 7
    VO = 0 if kind == "T0" else 2
    xT = nc.dram_tensor("xT", [D, T], F32, kind="ExternalInput").ap()
    vec = nc.dram_tensor("vec", [128, KC * NV], F32, kind="ExternalInput").ap()
    if kind == "Ta":
        OT = nc.dram_tensor("OT", [D, T], BF16, kind="ExternalInput").ap()
        wo = nc.dram_tensor("wo", [KC, 128, KC * 128], F32, kind="ExternalInput").ap()
    if kind == "Tb":
        yP = nc.dram_tensor("yP", [NE, D, T], BF16, kind="ExternalInput").ap()
        identb = nc.dram_tensor("identb", [128, 128], BF16, kind="ExternalInput").ap()
    if moe:
        rt = nc.dram_tensor("rt", [128, KC * NE], F32, kind="ExternalInput").ap()
        ident8 = nc.dram_tensor("ident8", [NE, NE], F32, kind="ExternalInput").ap()
        gout = nc.dram_tensor("gates", [T, NE], F32, kind="ExternalOutput").ap()
    if kind != "T0":
        xo = nc.dram_tensor("xo", [D, T], F32, kind="ExternalOutput").ap()
    hT = nc.dram_tensor("hT", [D, T], F32 if final else BF16, kind="ExternalOutput").ap()
    xv = xT.rearrange("(c p) t -> p c t", p=128)
    hv = hT.rearrange("(c p) t -> p c t", p=128)
    with contextlib.ExitStack() as st:
        sb = lambda n, s, d: st.enter_context(nc.sbuf_tensor(n, s, d))
        xs = sb("xs", [128, KC, T], F32)
        vs = sb("vs", [128, KC, NV], F32)
        amod = sb("amod", [128, KC, 2], F32); atmp = sb("atmp", [128, KC], F32)
        ones = sb("ones", [128, 128], F32); epst = sb("epst", [128, 1], F32)
        sq = [sb("sq%d" % i, [128, T], F32) for i in range(2)]
        rstd = sb("rstd", [128, T], F32); sd = sb("sd", [128, T], F32)
        xn = [sb("xn%d" % i, [128, T], F32) for i in range(2)]
        hf = [sb("hf%d" % i, [128, T], F32) for i in range(2)]
        ho = [sb("ho%d" % i, [128, T], F32 if final else BF16) for i in range(2)]
        ps = st.enter_context(nc.psum_tensor("ps", [128, 8, 512], F32))
        P = Prog(nc)
        outs = []
        P.d("sync", xs[:], xv, w=["x"])
        P.d("sync", vs[:].rearrange("p c v -> p (c v)"), vec, w=["vs"])
        P.e("vector", "memset", w=["ones"], ap=ones[:], constant=1.0 / D)
        P.e("vector", "memset", w=["eps"], ap=epst[:], constant=EPS)
        for s in range(2):
            P.e("vector", "tensor_scalar", r=["vs"], w=["atmp"], out=atmp[:], in0=vs[:, :, VO + 1 + s], scalar1=1.0, scalar2=None, op0=ALU.add)
            P.e("vector", "tensor_tensor", r=["atmp", "vs"], w=["amod"], out=amod[:, :, s], in0=atmp[:], in1=vs[:, :, VO], op=ALU.mult)

        def resid(c, pb):
            for (b, lo, hi, s) in PIECES:
                P.e("vector", "scalar_tensor_tensor", r=[("ps", pb + b), "vs", ("x", c)], w=[("x", c)],
                    out=xs[:, c, b * H + lo:b * H + hi], in0=ps[:, pb + b, lo:hi], scalar=vs[:, c, s:s + 1],
                    in1=xs[:, c, b * H + lo:b * H + hi], op0=ALU.mult, op1=ALU.add)

        if kind == "Ta":
            ot = sb("ot", [128, KC, T], BF16)
            wos = [sb("wos%d" % i, [128, KC * 128], BF16) for i in range(2)]
            P.d("sync", ot[:], OT.rearrange("(c p) t -> p c t", p=128), w=["ot"])
            P.d("gpsimd", wos[0][:], wo[0], w=[("wo", 0)])
            for oc in range(KC):
                wb = oc % 2
                if oc + 1 < KC:
                    P.d("gpsimd", wos[1 - wb][:], wo[oc + 1], w=[("wo", 1 - wb)])
                pb = 3 * (oc % 2)
                for kc in range(KC):
                    for b in range(3):
                        P.mm(ps[:, pb + b, 0:H], wos[wb][:, kc * 128:(kc + 1) * 128], ot[:, kc, b * H:(b + 1) * H], kc == 0, kc == KC - 1,
                             r=[("wo", wb), "ot"], w=[("ps", pb + b)])
                resid(oc, pb)
        if kind == "Tb":
            idb = sb("idb", [128, 128], BF16)
            pt = [sb("pt%d" % i, [128, NE, T], BF16) for i in range(2)]
            ypv = yP.rearrange("e (c p) t -> p c e t", p=128)
            P.d("sync", idb[:], identb, w=["idb"])
            P.d("sync", pt[0][:], ypv[:, 0], w=[("pt", 0)])
            for c in range(KC):
                tb = c % 2
                if c + 1 < KC:
                    P.d("sync", pt[1 - tb][:], ypv[:, c + 1], w=[("pt", 1 - tb)])
                pb = 3 * (c % 2)
                for e_ in range(NE):
                    for b in range(3):
                        P.mm(ps[:, pb + b, 0:H], idb[:], pt[tb][:, e_, b * H:(b + 1) * H], e_ == 0, e_ == NE - 1,
                             r=["idb", ("pt", tb)], w=[("ps", pb + b)])
                resid(c, pb)
        xtoks = ["x"] + [("x", c) for c in range(KC)]
        if kind != "T0":
            outs.append(P.d("sync", xo.rearrange("(c p) t -> p c t", p=128), xs[:], r=xtoks, w=["xo"]))
        for c in range(KC):
            P.e("scalar", "activation", r=["x", ("x", c)], w=[("sq", c % 2)], out=sq[c % 2][:], in_=xs[:, c, :], func=AF.Square)
            for b in range(3):
                P.mm(ps[:, b, 0:H], ones[:], sq[c % 2][:, b * H:(b + 1) * H], c == 0, c == KC - 1, r=["ones", ("sq", c % 2)], w=[("ps", b)])
        for b in range(3):
            P.e("scalar", "activation", r=[("ps", b), "eps"], w=["sd"], out=sd[:, b * H:(b + 1) * H], in_=ps[:, b, 0:H], func=AF.Sqrt, bias=epst[:, 0:1], scale=1.0)
        P.e("vector", "reciprocal", r=["sd"], w=["rstd"], out=rstd[:], in_=sd[:])
        if moe:
            rts = sb("rts", [128, KC, NE], F32); id8 = sb("id8", [NE, NE], F32)
            lg = sb("lg", [NE, T], F32)
            P.d("sync", rts[:].rearrange("p c v -> p (c v)"), rt, w=["rts"])
            P.d("sync", id8[:], ident8, w=["id8"])
        for c in range(KC):
            k = c % 2
            P.e("vector", "tensor_tensor", r=["x", ("x", c), "rstd"], w=[("xn", k)], out=xn[k][:], in0=xs[:, c, :], in1=rstd[:], op=ALU.mult)
            for s, (lo, hi) in enumerate(SEGS):
                P.e("vector", "tensor_scalar", r=[("xn", k), "amod", "vs"], w=[("hf", k)], out=hf[k][:, lo:hi], in0=xn[k][:, lo:hi],
                    scalar1=amod[:, c, s:s + 1], scalar2=vs[:, c, VO + 3 + s:VO + 4 + s], op0=ALU.mult, op1=ALU.add)
            if moe:
                for b in range(3):
                    P.mm(ps[0:NE, 3 + b, 0:H], rts[:, c, :], hf[k][:, b * H:(b + 1) * H], c == 0, c == KC - 1, r=["rts", ("hf", k)], w=[("ps", 3 + b)])
            P.e("scalar", "copy", r=[("hf", k)], w=[("ho", k)], out=ho[k][:], in_=hf[k][:])
            outs.append(P.d("sync", hv[:, c, :], ho[k][:], r=[("ho", k)], w=[("hout", c)]))
        if moe:
            NTL = T // 128
            L = sb("L", [128, NTL, NE], F32); m1 = sb("m1", [128, NTL], F32); nm1 = sb("nm1", [128, NTL], F32)
            eq = sb("eq", [128, NTL, NE], F32); L2 = sb("L2", [128, NTL, NE], F32); m2 = sb("m2", [128, NTL], F32)
            sel = sb("sel", [128, NTL, NE], F32); ex = sb("ex", [128, NTL, NE], F32); sm = sb("sm", [128, NTL], F32)
            gt = sb("gt", [128, NTL, NE], F32)
            for b in range(3):
                P.e("vector", "tensor_copy", r=[("ps", 3 + b)], w=["lg"], out=lg[:, b * H:(b + 1) * H], in_=ps[0:NE, 3 + b, 0:H])
            for t in range(NTL):
                P.mm(ps[:, 6, t * NE:(t + 1) * NE], lg[:, t * 128:(t + 1) * 128], id8[:], True, True, r=["lg", "id8"], w=[("ps", 6)])
            P.e("vector", "tensor_copy", r=[("ps", 6)], w=["L"], out=L[:].rearrange("p t e -> p (t e)"), in_=ps[:, 6, 0:NTL * NE])
            bc = lambda a: a[:].unsqueeze(2).to_broadcast([128, NTL, NE])
            P.e("vector", "tensor_reduce", r=["L"], w=["m1"], out=m1[:], in_=L[:], axis=AX.X, op=ALU.max)
            P.e("vector", "tensor_tensor", r=["L", "m1"], w=["eq"], out=eq[:], in0=L[:], in1=bc(m1), op=ALU.is_equal)
            P.e("vector", "scalar_tensor_tensor", r=["eq", "L"], w=["L2"], out=L2[:], in0=eq[:], scalar=-1e30, in1=L[:], op0=ALU.mult, op1=ALU.add)
            P.e("vector", "tensor_reduce", r=["L2"], w=["m2"], out=m2[:], in_=L2[:], axis=AX.X, op=ALU.max)
            P.e("vector", "tensor_tensor", r=["L", "m2"], w=["sel"], out=sel[:], in0=L[:], in1=bc(m2), op=ALU.is_ge)
            P.e("vector", "tensor_tensor", r=["L", "m1"], w=["ex"], out=ex[:], in0=L[:], in1=bc(m1), op=ALU.subtract)
            P.e("scalar", "activation", r=["ex"], w=["ex2"], out=eq[:], in_=ex[:], func=AF.Exp)
            P.e("vector", "tensor_tensor", r=["ex2", "sel"], w=["pp"], out=L2[:], in0=eq[:], in1=sel[:], op=ALU.mult)
            P.e("vector", "tensor_reduce", r=["pp"], w=["sm"], out=sm[:], in_=L2[:], axis=AX.X, op=ALU.add)
            P.e("vector", "reciprocal", r=["sm"], w=["rs"], out=m2[:], in_=sm[:])
            P.e("vector", "tensor_tensor", r=["pp", "rs"], w=["gt"], out=gt[:], in0=L2[:], in1=bc(m2), op=ALU.mult)
            outs.append(P.d("sync", gout.rearrange("(t p) e -> p t e", p=128), gt[:], r=["gt"], w=["gout"]))
        P.final_wait("sync", outs)
        P.run()
    return nc

def lay_vec(cols):
    v = np.stack([np.asarray(c, np.float32) for c in cols], 1)
    return np.ascontiguousarray(v.reshape(KC, 128, -1).transpose(1, 0, 2)).reshape(128, -1)


NEG = -30000.0

def nat_type(t):
    return 0 if t == 0 else 1 if t == 1 else 2 if t <= 13 else 3 if t == 14 else 4

def build_nat(NB=4):
    nc = bass.Bass("TRN2", target_bir_lowering=False)
    NTOK = NB * SB
    hT = nc.dram_tensor("hT", [D, NTOK], BF16, kind="ExternalInput").ap()
    wqk = nc.dram_tensor("wqk", [4, 128, KC * 128], F32, kind="ExternalInput").ap()
    wv = nc.dram_tensor("wv", [128, KC * 256], F32, kind="ExternalInput").ap()
    bias = nc.dram_tensor("bias", [128, 2 * 5 * 640], F32, kind="ExternalInput").ap()
    identb = nc.dram_tensor("identb", [128, 128], BF16, kind="ExternalInput").ap()
    O = nc.dram_tensor("O", [NTOK, 256], BF16, kind="ExternalOutput").ap()
    hv = hT.rearrange("(c p) t -> p c t", p=128)
    with contextlib.ExitStack() as st:
        sb = lambda n, s, d: st.enter_context(nc.sbuf_tensor(n, s, d))
        hs = sb("hs", [128, KC, SB], BF16)
        wqks = sb("wqks", [128, 4, KC * 128], BF16)
        wvs = sb("wvs", [128, KC * 256], BF16)
        bs = sb("bs", [128, 2, 5, 640], F32)
        idb = sb("idb", [128, 128], BF16)
        qk = sb("qk", [128, 4, SB], BF16)
        va = sb("va", [128, NTB, 2, DH + 1], BF16)
        ss = [sb("ss%d" % i, [128, 896], F32) for i in range(2)]
        pp = [sb("pp%d" % i, [128, 896], BF16) for i in range(2)]
        pT = [sb("pT%d" % i, [128, 7, 128], BF16) for i in range(2)]
        mx = sb("mx", [128, 2], F32); nmx = sb("nmx", [128, 2], F32); rc = sb("rc", [128, 2], F32)
        oo = [sb("oo%d" % i, [128, NTB, 256], BF16) for i in range(2)]
        ps = st.enter_context(nc.psum_tensor("ps", [128, 7, 512], F32))
        pst = st.enter_context(nc.psum_tensor("pst", [128, 1024], BF16))
        P = Prog(nc)
        outs = []
        for i in range(4):
            P.d("gpsimd", wqks[:, i, :], wqk[i], w=[("wqk", i)])
        P.d("gpsimd", wvs[:], wv, w=["wv"])
        P.d("sync", bs[:].rearrange("p a b k -> p (a b k)"), bias, w=["bias"])
        P.d("sync", idb[:], identb, w=["idb"])
        P.e("vector", "memset", w=["va1"], ap=va[:, :, :, DH:DH + 1], constant=1.0)
        scale = float(DH) ** -0.5
        u = 0
        for b in range(NB):
            P.d("sync", hs[:], hv[:, :, b * SB:(b + 1) * SB], w=["hs"])
            n = 0
            for blk in range(4):
                for tg in range(6):
                    pb = n % 2; n += 1
                    for kc in range(KC):
                        P.mm(ps[:, pb, 0:384], wqks[:, blk, kc * 128:(kc + 1) * 128], hs[:, kc, tg * 384:(tg + 1) * 384], kc == 0, kc == KC - 1,
                             r=[("wqk", blk), "hs"], w=[("ps", pb)])
                    if blk < 2:
                        P.e("scalar", "activation", r=[("ps", pb)], w=[("qk", blk)], out=qk[:, blk, tg * 384:(tg + 1) * 384], in_=ps[:, pb, 0:384], func=AF.Copy, scale=scale)
                    else:
                        P.e("vector", "tensor_copy", r=[("ps", pb)], w=[("qk", blk)], out=qk[:, blk, tg * 384:(tg + 1) * 384], in_=ps[:, pb, 0:384])
            for tl in range(NTB):
                pb = n % 2; n += 1
                for kc in range(KC):
                    P.mm(ps[:, pb, 0:256], hs[:, kc, tl * 128:(tl + 1) * 128], wvs[:, kc * 256:(kc + 1) * 256], kc == 0, kc == KC - 1,
                         r=["wv", "hs"], w=[("ps", pb)])
                P.e("vector", "tensor_copy", r=[("ps", pb)], w=["va"], out=va[:, tl, :, 0:DH], in_=ps[:, pb, 0:256].rearrange("p (j d) -> p j d", j=2))
            ob = b % 2
            for j in range(2):
                for qt in range(NTB):
                    k = u % 2; u += 1
                    bA, bB = 2 + 2 * k, 3 + 2 * k
                    lat = qt >= 2
                    if lat:
                        t = qt - 2
                        lt0 = min(max(t - 2, 0), 11)
                        k0 = (2 + lt0) * 128
                        ktiles = [2 + lt0 + i for i in range(5)] + [0, 1]
                        ty = nat_type(t)
                        P.mm(ps[:, bA, 0:512], qk[:, j, qt * 128:(qt + 1) * 128], qk[:, 2 + j, k0:k0 + 512], True, True, r=[("qk", j), ("qk", 2 + j)], w=[("ps", bA)])
                        P.mm(ps[:, bB, 0:128], qk[:, j, qt * 128:(qt + 1) * 128], qk[:, 2 + j, k0 + 512:k0 + 640], True, True, r=[("qk", j), ("qk", 2 + j)], w=[("ps", bB)])
                        P.mm(ps[:, bB, 128:384], qk[:, j, qt * 128:(qt + 1) * 128], qk[:, 2 + j, 0:256], True, True, r=[("qk", j), ("qk", 2 + j)], w=[("ps", bB)])
                        P.e("vector", "tensor_tensor", r=[("ps", bA), "bias"], w=[("ss", k)], out=ss[k][:, 0:512], in0=ps[:, bA, 0:512], in1=bs[:, j, ty, 0:512], op=ALU.add)
                        P.e("vector", "tensor_tensor", r=[("ps", bB), "bias"], w=[("ss", k)], out=ss[k][:, 512:640], in0=ps[:, bB, 0:128], in1=bs[:, j, ty, 512:640], op=ALU.add)
                        P.e("scalar", "copy", r=[("ps", bB)], w=[("ss", k)], out=ss[k][:, 640:896], in_=ps[:, bB, 128:384])
                        W = 896
                    else:
                        ktiles = [0, 1]
                        P.mm(ps[:, bB, 128:384], qk[:, j, qt * 128:(qt + 1) * 128], qk[:, 2 + j, 0:256], True, True, r=[("qk", j), ("qk", 2 + j)], w=[("ps", bB)])
                        P.e("scalar", "copy", r=[("ps", bB)], w=[("ss", k)], out=ss[k][:, 0:256], in_=ps[:, bB, 128:384])
                        W = 256
                    NK = len(ktiles)
                    P.e("vector", "tensor_reduce", r=[("ss", k)], w=[("mx", k)], out=mx[:, k:k + 1], in_=ss[k][:, 0:W], axis=AX.X, op=ALU.max)
                    P.e("vector", "tensor_scalar", r=[("mx", k)], w=[("nmx", k)], out=nmx[:, k:k + 1], in0=mx[:, k:k + 1], scalar1=-1.0, scalar2=None, op0=ALU.mult)
                    P.e("scalar", "activation", r=[("ss", k), ("nmx", k)], w=[("pp", k)], out=pp[k][:, 0:W], in_=ss[k][:, 0:W], func=AF.Exp, bias=nmx[:, k:k + 1], scale=1.0)
                    for i in range(NK):
                        P.op("tensor", lambda en, a=(pst[:, i * 128:(i + 1) * 128], pp[k][:, i * 128:(i + 1) * 128], idb[:]): en.transpose(a[0], a[1], a[2]),
                             reads=[("pp", k), "idb"], writes=["pst"])
                    P.e("scalar", "copy", r=["pst"], w=[("pT", k)], out=pT[k][:, 0:NK, :].rearrange("p a b -> p (a b)"), in_=pst[:, 0:NK * 128])
                    for i, kt in enumerate(ktiles):
                        P.mm(ps[:, 6, 0:DH + 1], pT[k][:, i, :], va[:, kt, j, :], i == 0, i == NK - 1, r=[("pT", k), "va", "va1"], w=[("ps", 6)])
                    P.e("vector", "reciprocal", r=[("ps", 6)], w=[("rc", k)], out=rc[:, k:k + 1], in_=ps[:, 6, DH:DH + 1])
                    P.e("vector", "tensor_scalar", r=[("ps", 6), ("rc", k)], w=[("oo", ob)], out=oo[ob][:, qt, j * DH:(j + 1) * DH], in0=ps[:, 6, 0:DH],
                        scalar1=rc[:, k:k + 1], scalar2=None, op0=ALU.mult)
            outs.append(P.d("sync", O[b * SB:(b + 1) * SB, :].rearrange("(t p) f -> p t f", p=128), oo[ob][:], r=[("oo", ob)], w=[("O", b)]))
        P.final_wait("sync", outs)
        P.run()
    return nc

def nat_bias_tiles(rpb2):
    out = np.full((128, 2, 5, 640), NEG, np.float32)
    qq = np.arange(128); kk = np.arange(640)
    for ty, t in enumerate((0, 1, 5, 14, 15)):
        base = 2 * min(max(t - 2, 0), 11)
        r = 2 * t + qq // 64; qc = qq % 64
        kr = base + kk // 64; kc_ = kk % 64
        rs = np.clip(r - 4, 0, 24); cs = np.clip(qc - 8, 0, 48)
        ok = (kr[None, :] >= rs[:, None]) & (kr[None, :] < rs[:, None] + 8) & (kc_[None, :] >= cs[:, None]) & (kc_[None, :] < cs[:, None] + 16)
        dr = np.clip(kr[None, :] - r[:, None] + 7, 0, 14); dc = np.clip(kc_[None, :] - qc[:, None] + 15, 0, 30)
        for j in range(2):
            out[:, j, ty, :] = np.where(ok, rpb2[j][dr, dc], NEG)
    return out.reshape(128, -1)


def build_gla(NB=2, dbg=9, sub=9):
    nc = bass.Bass("TRN2", target_bir_lowering=False)
    NT = NB * NTB
    hTt = nc.dram_tensor("hTt", [NT, 128, KC * 128], BF16, kind="ExternalInput").ap()
    wfm = nc.dram_tensor("wfm", [8, 128, KC * 128], F32, kind="ExternalInput").ap()
    wz = nc.dram_tensor("wz", [128, KC * 64], F32, kind="ExternalInput").ap()
    wvr = nc.dram_tensor("wvr", [128, KC * 1024], F32, kind="ExternalInput").ap()
    wgate = nc.dram_tensor("wgate", [16, 512], F32, kind="ExternalInput").ap()
    bgate = nc.dram_tensor("bgate", [128, 512], F32, kind="ExternalInput").ap()
    rope = nc.dram_tensor("rope", [128, 2 * 96], F32, kind="ExternalInput").ap()
    ng = nc.dram_tensor("ng", [128, DV], F32, kind="ExternalInput").ap()
    tri = nc.dram_tensor("tri", [128, 4 * 128], F32, kind="ExternalInput").ap()
    identf = nc.dram_tensor("identf", [128, 128], F32, kind="ExternalInput").ap()
    Og = nc.dram_tensor("Og", [NB * SB, DV], BF16, kind="ExternalOutput").ap()
    with contextlib.ExitStack() as st:
        sb = lambda n, s, d: st.enter_context(nc.sbuf_tensor(n, s, d))
        hs = [sb("hs0", [128, KC, 128], BF16)] * 2
        wfms = sb("wfms", [128, 8, KC * 128], BF16)
        wzs = sb("wzs", [128, KC * 64], BF16)
        wvrs = sb("wvrs", [128, KC * 1024], BF16)
        wgs = sb("wgs", [16, 512], F32); bgs = sb("bgs", [128, 512], F32)
        rp = sb("rp", [128, 2, 96], F32); ngs = sb("ngs", [128, DV], F32)
        tr = sb("tr", [128, 4, 128], F32); idf = sb("idf", [128, 128], F32)
        vs = sb("vs", [128, NTB, DV], BF16); rs = sb("rs", [128, NTB, DV], BF16)
        oacc = sb("oacc", [128, NTB, DV], F32)
        qpf = [sb("qpf%d" % i, [128, 2, 128], BF16) for i in range(2)]
        kpf = [sb("kpf%d" % i, [128, 2, 128], BF16) for i in range(2)]
        ktf = [sb("ktf%d" % i, [128, 256], BF16) for i in range(2)]
        qpb = sb("qpb", [128, NTB, 2, 128], BF16); kpb = sb("kpb", [128, NTB, 2, 128], BF16); ktb = sb("ktb", [128, NTB, 256], BF16)
        el = sb("el", [128, 2, NTB, 2], F32)
        S = sb("S", [128, 2, 2, DV], F32); Sb = sb("Sb", [128, 2, 2, DV], BF16)
        zsb = sb("zsb", [16, 2, 128], F32)
        qr = sb("qr", [128, 2, 128], F32); kr = sb("kr", [128, 2, 128], F32)
        t1 = sb("t1", [128, 128], F32); t2 = sb("t2", [128, 128], F32)
        scr = sb("scr", [128, 1024], F32); zp = scr[:, 0:256]; za = scr[:, 256:512]; ze = scr[:, 512:768]; gl = scr[:, 768:1024]
        Ep = sb("Ep", [128, 2, 128], F32); Em = sb("Em", [128, 2, 128], F32); kpf32 = sb("kpf32", [128, 2, 128], F32)
        at = sb("at", [128, 128], BF16)
        sqt = scr[:, 0:512]; ssum = sb("ssum", [128, 1], F32); rstd = sb("rstd", [128, 1], F32); t5 = scr[:, 512:1024]
        epst = sb("epst", [128, 1], F32)
        ps = st.enter_context(nc.psum_tensor("ps", [128, 8, 512], F32))
        P = Prog(nc)
        outs = []
        for i in range(8):
            P.d("gpsimd", wfms[:, i, :], wfm[i], w=["wfm"])
        P.d("gpsimd", wzs[:], wz, w=["wz"])
        P.d("gpsimd", wvrs[:], wvr, w=["wvr"])
        for (dst, src, tok) in ((wgs[:], wgate, "wg"), (bgs[:], bgate, "bg"), (rp[:].rearrange("p a b -> p (a b)"), rope, "rp"), (ngs[:], ng, "ng"),
                                (tr[:].rearrange("p a b -> p (a b)"), tri, "tr"), (idf[:], identf, "idf")):
            P.d("sync", dst, src, w=[tok])
        P.e("vector", "memset", w=["eps"], ap=epst[:], constant=EPS)
        P.d("sync", hs[0][:].rearrange("p c t -> p (c t)"), hTt[0], w=[("hs", 0)])
        HSINGLE = True

        def scan_step(d, tl, qp, kp, kt):
            rq = [("qp", d, tl), ("kp", d, tl)]
            for ch in range(2):
                P.mm(ps[:, 7, 0:128], kp[:, ch, :], qp[:, ch, :], ch == 0, ch == 1, r=rq, w=[("ps", 7)])
            P.e("vector", "tensor_tensor", r=[("ps", 7), "tr"], w=["at"], out=at[:], in0=ps[:, 7, 0:128], in1=tr[:, 2 + d, :], op=ALU.mult)
            if sub < 2: return
            P.mm(ps[:, 6, :], at[:], vs[:, tl, :], True, False, r=["at", ("v", tl)], w=[("ps", 6)])
            for ch in range(2):
                P.mm(ps[:, 6, :], qp[:, ch, :], Sb[:, d, ch, :], False, ch == 1, r=rq + [("Sb", d)], w=[("ps", 6)])
            if d == 0:
                P.e("scalar", "copy", r=[("ps", 6)], w=[("o", tl)], out=oacc[:, tl, :], in_=ps[:, 6, :])
            else:
                P.e("vector", "tensor_tensor", r=[("ps", 6), ("o", tl)], w=[("o", tl)], out=oacc[:, tl, :], in0=ps[:, 6, :], in1=oacc[:, tl, :], op=ALU.add)
            if sub < 3: return
            for ch in range(2):
                P.mm(ps[:, 7, :], kt[:, ch * 128:(ch + 1) * 128], vs[:, tl, :], True, True, r=[("kt", d, tl), ("v", tl)], w=[("ps", 7)])
                e_ = el[:, d, tl, ch:ch + 1]
                P.e("vector", "tensor_scalar", r=[("S", d), ("el", d, tl)], w=[("S", d)], out=S[:, d, ch, :], in0=S[:, d, ch, :], scalar1=e_, scalar2=None, op0=ALU.mult)
                P.e("vector", "scalar_tensor_tensor", r=[("ps", 7), ("S", d), ("el", d, tl)], w=[("S", d)], out=S[:, d, ch, :], in0=ps[:, 7, :], scalar=e_,
                    in1=S[:, d, ch, :], op0=ALU.mult, op1=ALU.add)
                P.e("scalar", "copy", r=[("S", d)], w=[("Sb", d)], out=Sb[:, d, ch, :], in_=S[:, d, ch, :])

        g = 0
        for b in range(NB):
            P.e("vector", "memset", w=[("S", 0), ("S", 1)], ap=S[:].rearrange("p a b c -> p (a b c)"), constant=0.0)
            P.e("vector", "memset", w=[("Sb", 0), ("Sb", 1)], ap=Sb[:].rearrange("p a b c -> p (a b c)"), constant=0.0)
            for tl in range(NTB):
                hb = 0
                if g > 0:
                    P.d("sync", hs[0][:].rearrange("p c t -> p (c t)"), hTt[g], w=[("hs", 0)])
                g += 1
                lat = tl >= 2; t = tl - 2
                for blk in range(8):
                    for kc in range(KC):
                        P.mm(ps[:, blk // 4, (blk % 4) * 128:(blk % 4 + 1) * 128], wfms[:, blk, kc * 128:(kc + 1) * 128], hs[hb][:, kc, :], kc == 0, kc == KC - 1,
                             r=["wfm", ("hs", hb)], w=[("ps", "pq", blk)])
                for d in range(2):
                    for kc in range(KC):
                        P.mm(ps[0:16, 2, 0:128] if d == 0 else ps[0:16, 5, 384:512],
                             wzs[:, kc * 64 + 32 * d:kc * 64 + 32 * d + 16], hs[hb][:, kc, :], kc == 0, kc == KC - 1, r=["wz", ("hs", hb)], w=[("ps", 2, "z") if d == 0 else ("ps", 5, "b")])
                for (bank, off) in ((3, 0), (4, 512)):
                    for kc in range(KC):
                        P.mm(ps[:, bank, :], hs[hb][:, kc, :], wvrs[:, kc * 1024 + off:kc * 1024 + off + 512], kc == 0, kc == KC - 1, r=["wvr", ("hs", hb)], w=[("ps", bank)])
                P.e("scalar", "copy", r=[("ps", 3)], w=[("v", tl)], out=vs[:, tl, :], in_=ps[:, 3, :])
                P.e("scalar", "activation", r=[("ps", 4)], w=[("r", tl)], out=rs[:, tl, :], in_=ps[:, 4, :], func=AF.Silu)
                P.e("vector", "tensor_copy", r=[("ps", 2, "z")], w=["zsb"], out=zsb[:, 0, :], in_=ps[0:16, 2, 0:128])
                P.e("vector", "tensor_copy", r=[("ps", 5, "b")], w=["zsb"], out=zsb[:, 1, :], in_=ps[0:16, 5, 384:512])
                if dbg < 2: continue
                for (dst, tok, b0) in ((qr, "qr", 0), (kr, "kr", 4)):
                    for ch in range(2):
                        src = ps[:, (b0 + ch) // 4, ((b0 + ch) % 4) * 128:((b0 + ch) % 4 + 1) * 128]
                        ssw = ps[:, (b0 + 2 + ch) // 4, ((b0 + 2 + ch) % 4) * 128:((b0 + 2 + ch) % 4 + 1) * 128]
                        rd = [("ps", "pq", b0 + ch), ("ps", "pq", b0 + 2 + ch)]
                        if lat:
                            v3 = lambda a: a.rearrange("p (a b) -> p a b", a=2)
                            if ch == 0:
                                cb = rp[:, 0, 2 * t:2 * t + 2].unsqueeze(2).to_broadcast([128, 2, 64]); sbb = rp[:, 1, 2 * t:2 * t + 2].unsqueeze(2).to_broadcast([128, 2, 64])
                            else:
                                cb = rp[:, 0, 32:96].unsqueeze(1).to_broadcast([128, 2, 64]); sbb = rp[:, 1, 32:96].unsqueeze(1).to_broadcast([128, 2, 64])
                            P.e("vector", "tensor_tensor", r=rd + ["rp"], w=["t1"], out=v3(t1[:]), in0=v3(src), in1=cb, op=ALU.mult)
                            P.e("vector", "tensor_tensor", r=rd + ["rp"], w=["t2"], out=v3(t2[:]), in0=v3(ssw), in1=sbb, op=ALU.mult)
                            P.e("vector", "tensor_tensor", r=["t1", "t2"], w=[tok], out=dst[:, ch, :], in0=t1[:], in1=t2[:], op=ALU.add)
                        else:
                            P.e("vector", "tensor_copy", r=rd, w=[tok], out=dst[:, ch, :], in_=src)
                if dbg < 3: continue
                for d in range(2):
                    P.mm(ps[:, 5, 0:256], zsb[:, d, :], wgs[:, d * 256:(d + 1) * 256], True, True, r=["zsb", "wg"], w=[("ps", 5, "z")])
                    P.e("vector", "tensor_tensor", r=[("ps", 5, "z"), "bg"], w=["scr"], out=zp, in0=ps[:, 5, 0:256], in1=bgs[:, d * 256:(d + 1) * 256], op=ALU.add)
                    P.e("vector", "scalar_tensor_tensor", r=["scr"], w=["scr"], out=za, in0=zp, scalar=-1.0, in1=zp, op0=ALU.mult, op1=ALU.max)
                    P.e("scalar", "activation", r=["scr"], w=["scr"], out=ze, in_=za, func=AF.Exp, scale=-1.0)
                    P.e("scalar", "activation", r=["scr"], w=["scr"], out=za, in_=ze, func=AF.Ln, bias=1.0, scale=1.0)
                    P.e("vector", "tensor_scalar", r=["scr"], w=["scr"], out=ze, in0=zp, scalar1=0.0, scalar2=None, op0=ALU.min)
                    P.e("vector", "tensor_tensor", r=["scr", "scr"], w=["scr"], out=gl, in0=ze, in1=za, op=ALU.subtract)
                    for ch in range(2):
                        P.mm(ps[:, 5, 256 + ch * 128:256 + (ch + 1) * 128], gl[:, ch * 128:(ch + 1) * 128], tr[:, d, :], True, True, r=["scr", "tr"], w=[("ps", 5, "b")])
                    P.e("scalar", "activation", r=[("ps", 5, "b")], w=["Ep"], out=Ep[:].rearrange("p a b -> p (a b)"), in_=ps[:, 5, 256:512], func=AF.Exp)
                    P.e("scalar", "activation", r=[("ps", 5, "b")], w=["Em"], out=Em[:].rearrange("p a b -> p (a b)"), in_=ps[:, 5, 256:512], func=AF.Exp, scale=-1.0)
                    col = 127 if d == 0 else 0
                    P.e("vector", "tensor_copy", r=["Ep"], w=[("el", d, tl)], out=el[:, d, tl, :], in_=Ep[:, :, col])
                    qp = qpf[tl % 2] if d == 0 else qpb[:, tl]
                    kp = kpf[tl % 2] if d == 0 else kpb[:, tl]
                    kt = ktf[tl % 2] if d == 0 else ktb[:, tl]
                    qpv = qp[:] if d == 0 else qp
                    kpv = kp[:] if d == 0 else kp
                    ktv = kt[:] if d == 0 else kt
                    P.e("vector", "scalar_tensor_tensor", r=["qr", "Ep"], w=[("qp", d, tl)], out=qpv, in0=qr[:], scalar=1.0 / 16.0, in1=Ep[:], op0=ALU.mult, op1=ALU.mult)
                    P.e("vector", "tensor_tensor", r=["kr", "Em"], w=["kpf32"], out=kpf32[:], in0=kr[:], in1=Em[:], op=ALU.mult)
                    P.e("scalar", "copy", r=["kpf32"], w=[("kp", d, tl)], out=kpv, in_=kpf32[:])
                    for ch in range(2):
                        P.mm(ps[:, 2, 256 + ch * 128:256 + (ch + 1) * 128], kpf32[:, ch, :], idf[:], True, True, r=["kpf32", "idf"], w=[("ps", 2, "kt")])
                    P.e("vector", "tensor_copy", r=[("ps", 2, "kt")], w=[("kt", d, tl)], out=ktv, in_=ps[:, 2, 256:512])
                    if d == 0 and dbg >= 4:
                        scan_step(0, tl, qpv, kpv, ktv)
            for tl in ([1, 0] + list(range(NTB - 1, 1, -1)) if dbg >= 5 else []):
                scan_step(1, tl, qpb[:, tl], kpb[:, tl], ktb[:, tl])
            for tl in (range(NTB) if dbg >= 6 else []):
                P.e("scalar", "activation", r=[("o", tl)], w=["scr"], out=sqt, in_=oacc[:, tl, :], func=AF.Square)
                P.e("vector", "tensor_reduce", r=["scr"], w=["ssum"], out=ssum[:], in_=sqt, axis=AX.X, op=ALU.add)
                P.e("scalar", "activation", r=["ssum", "eps"], w=["rstd0"], out=rstd[:], in_=ssum[:], func=AF.Sqrt, bias=epst[:, 0:1], scale=1.0 / DV)
                P.e("vector", "reciprocal", r=["rstd0"], w=["rstd"], out=ssum[:], in_=rstd[:])
                P.e("vector", "scalar_tensor_tensor", r=[("o", tl), "rstd", "ng"], w=["scr"], out=t5, in0=oacc[:, tl, :], scalar=ssum[:, 0:1], in1=ngs[:], op0=ALU.mult, op1=ALU.mult)
                P.e("vector", "tensor_tensor", r=["scr", ("r", tl)], w=[("r", tl)], out=rs[:, tl, :], in0=t5, in1=rs[:, tl, :], op=ALU.mult)
            outs.append(P.d("sync", Og[b * SB:(b + 1) * SB, :].rearrange("(t p) f -> p t f", p=128), rs[:], r=[("r", tl) for tl in range(NTB)], w=[("Og", b)]))
        P.final_wait("sync", outs)
        P.run()
    return nc

def gla_consts():
    half = 128
    freqs = 10000.0 ** (-np.arange(0, half, 2, dtype=np.float32) / half)
    fr = np.concatenate([freqs, freqs])
    pos = np.concatenate([np.arange(32), np.arange(64)]).astype(np.float32)
    ang = fr[:, None] * pos[None, :]
    sgn = np.where(np.arange(128) < 64, -1.0, 1.0)[:, None]
    rope = np.concatenate([np.cos(ang), sgn * np.sin(ang)], 1).astype(np.float32)
    j = np.arange(128)[:, None]; i = np.arange(128)[None, :]
    U = (j <= i).astype(np.float32); L = (j >= i).astype(np.float32)
    tri = np.concatenate([U / 16.0, L / 16.0, U, L], 1).astype(np.float32)
    return rope, tri, np.eye(128, dtype=np.float32)


def build_ada():
    nc = bass.Bass("TRN2", target_bir_lowering=False)
    cT = nc.dram_tensor("cT", [128, KC * NR], F32, kind="ExternalInput").ap()
    w = nc.dram_tensor("w", [NL * 4, 128, 4 * NCOL], F32, kind="ExternalInput").ap()
    bia = nc.dram_tensor("bia", [1, NL * NCOL], F32, kind="ExternalInput").ap()
    m = nc.dram_tensor("m", [NR, NL * NCOL], F32, kind="ExternalOutput").ap()
    with contextlib.ExitStack() as st:
        sb = lambda n, s, d: st.enter_context(nc.sbuf_tensor(n, s, d))
        cs = sb("cs", [128, KC, NR], F32); sc = sb("sc", [128, KC, NR], F32)
        ws = [sb("ws%d" % i, [128, 4, NCOL], F32) for i in range(2)]
        bs = sb("bs", [1, NL * NCOL], F32); ones = sb("ones", [1, NR], F32)
        mo = sb("mo", [NR, NL * NCOL], F32)
        ps = st.enter_context(nc.psum_tensor("ps", [128, 8, 512], F32))
        P = Prog(nc)
        P.d("sync", cs[:].rearrange("p c r -> p (c r)"), cT, w=["cs"])
        P.d("sync", bs[:], bia, w=["bs"])
        P.e("vector", "memset", w=["ones"], ap=ones[:], constant=1.0)
        P.e("scalar", "activation", r=["cs"], w=["sc"], out=sc[:].rearrange("p c r -> p (c r)"), in_=cs[:].rearrange("p c r -> p (c r)"), func=AF.Silu)
        P.d("sync", ws[0][:].rearrange("p k c -> p (k c)"), w[0], w=[("ws", 0)])
        n = 0
        for l in range(NL):
            pb = 3 * (l % 2)
            for kg in range(4):
                b = n % 2; n += 1
                if n < NL * 4:
                    P.d("sync", ws[1 - b][:].rearrange("p k c -> p (k c)"), w[n], w=[("ws", 1 - b)])
                for k4 in range(4):
                    kc = kg * 4 + k4
                    for j in range(3):
                        P.mm(ps[0:NR, pb + j, :], sc[:, kc, :], ws[b][:, k4, j * 512:(j + 1) * 512], kc == 0, False, r=["sc", ("ws", b)], w=[("ps", pb + j)])
            for j in range(3):
                P.mm(ps[0:NR, pb + j, :], ones[:], bs[:, l * NCOL + j * 512:l * NCOL + (j + 1) * 512], False, True, r=["ones", "bs"], w=[("ps", pb + j)])
                P.e("vector", "tensor_copy", r=[("ps", pb + j)], w=["mo"], out=mo[:, l * NCOL + j * 512:l * NCOL + (j + 1) * 512], in_=ps[0:NR, pb + j, :])
        o = P.d("sync", m, mo[:], r=["mo"], w=["m"])
        P.final_wait("sync", [o])
        P.run()
    return nc

def lay_ada(ada_w, ada_b, c, c_ctx, core):
    cols = slice(core * NCOL, (core + 1) * NCOL)
    w = ada_w[:, :, cols]
    w = np.ascontiguousarray(w.reshape(NL, 4, 4, 128, NCOL).transpose(0, 1, 3, 2, 4)).reshape(NL * 4, 128, 4 * NCOL)
    cc = np.concatenate([c, c_ctx[None]], 0)
    cT = np.ascontiguousarray(cc.T.reshape(KC, 128, NR).transpose(1, 0, 2)).reshape(128, KC * NR)
    return {"cT": cT, "w": w, "bia": np.ascontiguousarray(ada_b[:, cols]).reshape(1, NL * NCOL)}


NCORES = 8
NTOK = 9216
_PROGS = {}


def _prog(key, fn):
    if key not in _PROGS:
        _PROGS[key] = fn()
    return _PROGS[key]


def _run(nc, in_maps):
    res = run_bass_kernel_spmd(nc, in_maps, core_ids=list(range(NCORES)))
    return res.results


def _bf(a):
    return np.ascontiguousarray(a)


def gla_inputs(hT_bf, wq, wk, wv, wr, wzf, wzb, wgf, wgb, bgf, bgb, ngv):
    NT = hT_bf.shape[1] // 128
    hTt = np.ascontiguousarray(hT_bf.reshape(KC, 128, NT, 128).transpose(2, 1, 0, 3)).reshape(NT, 128, KC * 128)
    sw = np.concatenate([np.arange(64, 128), np.arange(0, 64)])
    perm = np.concatenate([sw, 128 + sw])
    blocks = np.concatenate([wq, wq[:, perm], wk, wk[:, perm]], 1)
    wfm = lay_w1(blocks, 8)
    wzp = np.zeros((D, 64), np.float32); wzp[:, 0:16] = wzf; wzp[:, 32:48] = wzb
    wz = np.ascontiguousarray(wzp.reshape(KC, 128, 64).transpose(1, 0, 2)).reshape(128, KC * 64)
    wvr = np.ascontiguousarray(np.concatenate([wv, wr], 1).reshape(KC, 128, 1024).transpose(1, 0, 2)).reshape(128, KC * 1024)
    wgate = np.ascontiguousarray(np.concatenate([wgf, wgb], 1)).astype(np.float32)
    bgate = np.ascontiguousarray(np.broadcast_to(np.concatenate([bgf, bgb])[None, :], (128, 512))).astype(np.float32)
    rope, tri, idf = gla_consts()
    return {"hTt": hTt, "wfm": wfm, "wz": wz, "wvr": wvr, "wgate": wgate, "bgate": bgate, "rope": rope,
            "ng": np.ascontiguousarray(np.broadcast_to(ngv[None, :], (128, 512))).astype(np.float32), "tri": tri, "identf": idf}


def kernel(x, c, ctx, c_ctx, ada_w, ada_b, norm_mix_g, norm_ffn_g, gla_w_in, gla_wg_fwd, gla_bg_fwd, gla_wg_bwd, gla_bg_bwd,
           gla_norm_g, gla_w_out, nat_w_in, nat_rpb, nat_w_out, ffn_w_gate, ffn_w_up, ffn_w_down, moe_router, moe_w_gate,
           moe_w_up, moe_w_down, final_norm_g, _probe=None):
    A = lambda a: np.asarray(a, np.float32)
    x, c, ctx, c_ctx = A(x), A(c), A(ctx), A(c_ctx)
    probe = _probe or (lambda name, val: None)
    identb = np.eye(128, dtype=np.float32).astype(ml_dtypes.bfloat16)
    ident8 = np.eye(8, dtype=np.float32)
    zeros = np.zeros(D, np.float32)
    res = _run(_prog("ada", build_ada), [lay_ada(A(ada_w), A(ada_b), c, c_ctx, k) for k in range(NCORES)])
    M = np.concatenate([r["m"].reshape(NR, NL, NCOL) for r in res], 2)
    probe("ada", M)

    def mods(layer, core):
        b = core // 2
        rows = (4 if core % 2 == 0 else b, b)
        return [[M[r, layer, j * D:(j + 1) * D] for r in rows] for j in range(6)]

    X = np.concatenate([np.concatenate([ctx[b], x[b]], 0) for b in range(4)], 0)
    xT = [np.ascontiguousarray(X[1152 * k:1152 * (k + 1)].T) for k in range(NCORES)]
    ims = []
    for k in range(NCORES):
        sh1, sc1, g1, sh2, sc2, g2 = mods(0, k)
        ims.append({"xT": xT[k], "vec": lay_vec([A(norm_mix_g)[0], sc1[0], sc1[1], sh1[0], sh1[1]])})
    res = _run(_prog("T0", lambda: build_tok("T0")), ims)
    HT = np.concatenate([r["hT"] for r in res], 1)
    probe("h_0", HT)
    for i in range(4):
        j = i // 2
        last = i == 3
        if i % 2 == 0:
            w_in = A(gla_w_in)[j]
            q_, k_, v_, r_ = w_in[:, 0:1024], w_in[:, 1024:2048], w_in[:, 2048:4096], w_in[:, 4096:6144]
            zf_, zb_ = w_in[:, 6144:6160], w_in[:, 6160:6176]
            ims = []
            for k in range(NCORES):
                hd, bg = k % 4, k // 4
                ims.append(gla_inputs(np.ascontiguousarray(HT[:, 4608 * bg:4608 * (bg + 1)]),
                                      q_[:, 256 * hd:256 * (hd + 1)], k_[:, 256 * hd:256 * (hd + 1)], v_[:, 512 * hd:512 * (hd + 1)],
                                      r_[:, 512 * hd:512 * (hd + 1)], zf_, zb_,
                                      A(gla_wg_fwd)[j][:, 256 * hd:256 * (hd + 1)], A(gla_wg_bwd)[j][:, 256 * hd:256 * (hd + 1)],
                                      A(gla_bg_fwd)[j][256 * hd:256 * (hd + 1)], A(gla_bg_bwd)[j][256 * hd:256 * (hd + 1)], A(gla_norm_g)[j]))
            res = _run(_prog("gla", lambda: build_gla(2)), ims)
            O = np.empty((NTOK, D), ml_dtypes.bfloat16)
            for k in range(NCORES):
                hd, bg = k % 4, k // 4
                O[4608 * bg:4608 * (bg + 1), 512 * hd:512 * (hd + 1)] = res[k]["Og"]
            w_out = A(gla_w_out)[j]
        else:
            w_in = A(nat_w_in)[j]
            ims = []
            for k in range(NCORES):
                cs = slice(256 * k, 256 * (k + 1))
                wqk = np.concatenate([lay_w1(w_in[:, 0:2048][:, cs], 2), lay_w1(w_in[:, 2048:4096][:, cs], 2)], 0)
                wv = np.ascontiguousarray(w_in[:, 4096:6144][:, cs].reshape(KC, 128, 256).transpose(1, 0, 2)).reshape(128, KC * 256)
                ims.append({"hT": HT, "wqk": wqk, "wv": wv, "bias": nat_bias_tiles(A(nat_rpb)[j][2 * k:2 * k + 2]), "identb": identb})
            res = _run(_prog("nat", lambda: build_nat(4)), ims)
            O = np.concatenate([r["O"] for r in res], 1)
            w_out = A(nat_w_out)[j]
        probe("O_%d" % i, O)
        moe = i % 2 == 1
        wo_l = lay_w1(w_out, KC)
        ims = []
        for k in range(NCORES):
            sh1, sc1, g1, sh2, sc2, g2 = mods(i, k)
            im = {"xT": xT[k], "OT": np.ascontiguousarray(O[1152 * k:1152 * (k + 1)].T), "wo": wo_l,
                  "vec": lay_vec([g1[0], g1[1], A(norm_ffn_g)[i], sc2[0], sc2[1], sh2[0], sh2[1]])}
            if moe:
                rt = A(moe_router)[j]
                im["rt"] = lay_vec([rt[:, e] for e in range(8)]); im["ident8"] = ident8
            ims.append(im)
        res = _run(_prog("Ta%d" % moe, lambda: build_tok("Ta", moe=moe)), ims)
        xT = [r["xo"] for r in res]
        H2T = np.concatenate([r["hT"] for r in res], 1)
        probe("xa_%d" % i, xT); probe("h2_%d" % i, H2T)
        ims = []
        if moe:
            G = np.concatenate([r["gates"] for r in res], 0)
            probe("gates_%d" % i, G)
            for k in range(NCORES):
                ims.append({"hT": H2T, "wg": lay_w1(A(moe_w_gate)[j][k], 44), "wu": lay_w1(A(moe_w_up)[j][k], 44),
                            "wd": lay_w2(A(moe_w_down)[j][k], 44), "gate": np.ascontiguousarray(G[:, k]).reshape(1, NTOK)})
            res = _run(_prog("ffn44", lambda: build_ffn(NTOK, 44)), ims)
        else:
            ones = np.ones((1, NTOK), np.float32)
            for k in range(NCORES):
                fs = slice(704 * k, 704 * (k + 1))
                ims.append({"hT": H2T, "wg": lay_w1(A(ffn_w_gate)[j][:, fs], 6), "wu": lay_w1(A(ffn_w_up)[j][:, fs], 6),
                            "wd": lay_w2(A(ffn_w_down)[j][fs, :], 6), "gate": ones})
            res = _run(_prog("ffn6", lambda: build_ffn(NTOK, 6)), ims)
        YP = [r["yT"] for r in res]
        probe("yp_%d" % i, YP)
        ims = []
        for k in range(NCORES):
            sh1, sc1, g1, sh2, sc2, g2 = mods(i, k)
            if last:
                cols = [g2[0], g2[1], A(final_norm_g), zeros, zeros, zeros, zeros]
            else:
                nsh1, nsc1 = mods(i + 1, k)[0], mods(i + 1, k)[1]
                cols = [g2[0], g2[1], A(norm_mix_g)[i + 1], nsc1[0], nsc1[1], nsh1[0], nsh1[1]]
            yp = np.stack([np.ascontiguousarray(YP[e][:, 1152 * k:1152 * (k + 1)]) for e in range(NCORES)], 0)
            ims.append({"xT": xT[k], "yP": yp, "identb": identb, "vec": lay_vec(cols)})
        res = _run(_prog("Tb%d" % last, lambda: build_tok("Tb", final=last)), ims)
        xT = [r["xo"] for r in res]
        HT = np.concatenate([r["hT"] for r in res], 1)
        probe("xb_%d" % i, xT)
        if not last:
            probe("h_%d" % (i + 1), HT)
    out_all = np.ascontiguousarray(HT.T).astype(np.float32)
    return np.stack([out_all[2304 * b + 256:2304 * (b + 1)] for b in range(4)], 0)
```

```python
import contextlib
import numpy as np
import ml_dtypes
import concourse.bass as bass
import concourse.mybir as mybir
from concourse.bass_utils import run_bass_kernel_spmd

F32 = mybir.dt.float32; BF16 = mybir.dt.bfloat16
AF = mybir.ActivationFunctionType; ALU = mybir.AluOpType; AX = mybir.AxisListType
D = 2048; KC = 16; EPS = 1e-6
T = 1152; H = 384; NE = 8
SB = 2304; NTB = 18; DH = 128; DV = 512
NL = 4; NCOL = 1536; NR = 5


ENGS = ("tensor", "vector", "scalar", "gpsimd", "sync")


class Prog:
    def __init__(self, nc, n_dma_sems=24):
        self.nc = nc
        self.ops = []
        self.cnt = {e: 0 for e in ENGS}
        self.last_w = {}
        self.readers = {}
        self.waited = {e: {} for e in ENGS}
        self.n_dma_sems = n_dma_sems
        self.dma_cnt = [0] * n_dma_sems
        self.dma_busy = [None] * n_dma_sems

    def _deps(self, eng, reads, writes):
        need = []
        for t in reads:
            if t in self.last_w:
                need.append(self.last_w[t])
        for t in writes:
            if t in self.last_w:
                need.append(self.last_w[t])
            need.extend(self.readers.get(t, ()))
        out = {}
        for key, c in need:
            if c > self.waited[eng].get(key, 0) and c > out.get(key, 0):
                out[key] = c
        for key, c in out.items():
            self.waited[eng][key] = c
        return sorted(out.items(), key=lambda kv: str(kv[0]))

    def _commit(self, comp, reads, writes):
        for t in reads:
            self.readers.setdefault(t, []).append(comp)
        for t in writes:
            self.last_w[t] = comp
            self.readers[t] = []

    def op(self, eng, emit, reads=(), writes=()):
        waits = self._deps(eng, reads, writes)
        self.cnt[eng] += 1
        comp = (("e", eng), self.cnt[eng])
        self.ops.append(dict(eng=eng, emit=emit, waits=waits, inc=comp))
        self._commit(comp, reads, writes)
        return comp

    def dma(self, eng, emit, reads=(), writes=(), slot=None):
        if slot is None:
            slot = min(range(self.n_dma_sems), key=lambda i: self.dma_cnt[i])
        waits = dict(self._deps(eng, reads, writes))
        prev = self.dma_busy[slot]
        if prev is not None and prev[1] > self.waited[eng].get(prev[0], 0):
            waits[prev[0]] = max(waits.get(prev[0], 0), prev[1])
            self.waited[eng][prev[0]] = prev[1]
        self.dma_cnt[slot] += 1
        comp = (("d", slot), 16 * self.dma_cnt[slot])
        self.dma_busy[slot] = comp
        self.ops.append(dict(eng=eng, emit=emit, waits=sorted(waits.items(), key=lambda kv: str(kv[0])), inc=comp, dma=True))
        self._commit(comp, reads, writes)
        return comp

    def e(self, eng, name, r=(), w=(), **kw):
        return self.op(eng, lambda en, name=name, kw=kw: getattr(en, name)(**kw), reads=r, writes=w)

    def mm(self, out, lhsT, rhs, start, stop, r=(), w=()):
        return self.op("tensor", lambda en, a=(out, lhsT, rhs), s=(start, stop): en.matmul(a[0], a[1], a[2], start=s[0], stop=s[1]), reads=r, writes=w)

    def d(self, eng, out, in_, r=(), w=(), slot=None):
        return self.dma(eng, lambda en, a=(out, in_): en.dma_start(out=a[0], in_=a[1]), reads=r, writes=w, slot=slot)

    def final_wait(self, eng, comps):
        w = {}
        for key, c in comps:
            w[key] = max(w.get(key, 0), c)
        self.ops.append(dict(eng=eng, emit=None, waits=sorted(w.items(), key=lambda kv: str(kv[0])), inc=None))

    def run(self):
        nc = self.nc
        with contextlib.ExitStack() as st:
            esem = {e: st.enter_context(nc.semaphore("se_" + e)) for e in ENGS}
            dsem = [st.enter_context(nc.semaphore("sd_%d" % i)) for i in range(self.n_dma_sems)]
            block = st.enter_context(nc.Block())

            def sem_of(key):
                return esem[key[1]] if key[0] == "e" else dsem[key[1]]

            def replay(engname):
                def body(e):
                    for o in self.ops:
                        if o["eng"] != engname:
                            continue
                        for key, c in o["waits"]:
                            e.wait_ge(sem_of(key), c)
                        if o["emit"] is None:
                            continue
                        ins = o["emit"](e)
                        key, c = o["inc"]
                        ins.then_inc(sem_of(key), 16 if key[0] == "d" else 1)
                return body

            block.tensor(replay("tensor"))
            block.vector(replay("vector"))
            block.scalar(replay("scalar"))
            block.gpsimd(replay("gpsimd"))
            block.sync(replay("sync"))


def build_ffn(NTOK, NFC, TT=768):
    assert TT == 768 and NTOK % TT == 0
    NT = NTOK // TT; H = TT // 2
    nc = bass.Bass("TRN2", target_bir_lowering=False)
    hT = nc.dram_tensor("hT", [D, NTOK], BF16, kind="ExternalInput").ap()
    wg = nc.dram_tensor("wg", [NFC, 128, KC * 128], F32, kind="ExternalInput").ap()
    wu = nc.dram_tensor("wu", [NFC, 128, KC * 128], F32, kind="ExternalInput").ap()
    wd = nc.dram_tensor("wd", [KC, 128, NFC * 128], F32, kind="ExternalInput").ap()
    gate = nc.dram_tensor("gate", [1, NTOK], F32, kind="ExternalInput").ap()
    yT = nc.dram_tensor("yT", [D, NTOK], BF16, kind="ExternalOutput").ap()
    hv = hT.rearrange("(c p) t -> p c t", p=128)
    yv = yT.rearrange("(c p) t -> p c t", p=128)
    with contextlib.ExitStack() as st:
        sb = lambda n, s, d: st.enter_context(nc.sbuf_tensor(n, s, d))
        hs = [sb("hs%d" % i, [128, KC, TT], BF16) for i in range(2)]
        act = sb("act", [128, NFC, TT], BF16)
        wgs = [sb("wgs%d" % i, [128, KC * 128], BF16) for i in range(2)]
        wus = [sb("wus%d" % i, [128, KC * 128], BF16) for i in range(2)]
        wds = [sb("wds%d" % i, [128, NFC * 128], BF16) for i in range(2)]
        g1 = sb("g1", [1, TT], F32); ones = sb("ones", [1, 128], F32)
        gbc = sb("gbc", [128, TT], F32)
        sg = [sb("sg%d" % i, [128, TT], F32) for i in range(2)]
        yo = [sb("yo%d" % i, [128, TT], BF16) for i in range(2)]
        ps = st.enter_context(nc.psum_tensor("ps", [128, 8, 512], F32))
        P = Prog(nc)
        P.e("vector", "memset", w=["ones"], ap=ones[:], constant=1.0)
        outs = []
        def load_h(tt):
            P.d("sync", hs[tt % 2][:], hv[:, :, tt * TT:(tt + 1) * TT], w=[("hs", tt % 2)])
        def load_w1(fc, b):
            P.d("gpsimd", wgs[b][:], wg[fc], w=[("wg", b)])
            P.d("gpsimd", wus[b][:], wu[fc], w=[("wu", b)])
        def load_w2(oc, b):
            P.d("gpsimd", wds[b][:], wd[oc], w=[("wd", b)])
        w1n = 0; w2n = 0
        load_h(0)
        load_w1(0, 0)
        for tt in range(NT):
            hb = tt % 2
            if tt + 1 < NT:
                load_h(tt + 1)
            P.d("sync", g1[:], gate[:, tt * TT:(tt + 1) * TT], w=["g1"])
            for hf in range(2):
                P.mm(ps[:, hf, 0:H], ones[:], g1[:, hf * H:(hf + 1) * H], True, True, r=["ones", "g1"], w=[("ps", hf)])
                P.e("vector", "tensor_copy", r=[("ps", hf)], w=["gbc"], out=gbc[:, hf * H:(hf + 1) * H], in_=ps[:, hf, 0:H])
            for fc in range(NFC):
                wb = w1n % 2; w1n += 1
                if fc + 1 < NFC:
                    load_w1(fc + 1, w1n % 2)
                else:
                    load_w2(0, w2n % 2)
                s = fc % 2
                for (wt, tok, base) in ((wgs, "wg", 4 * s), (wus, "wu", 4 * s + 2)):
                    for kc in range(KC):
                        for hf in range(2):
                            P.mm(ps[:, base + hf, 0:H], wt[wb][:, kc * 128:(kc + 1) * 128], hs[hb][:, kc, hf * H:(hf + 1) * H],
                                 kc == 0, kc == KC - 1, r=[(tok, wb), ("hs", hb)], w=[("ps", base + hf)])
                for hf in range(2):
                    P.e("scalar", "activation", r=[("ps", 4 * s + hf)], w=[("sg", s, hf)],
                        out=sg[s][:, hf * H:(hf + 1) * H], in_=ps[:, 4 * s + hf, 0:H], func=AF.Silu)
                    P.e("vector", "tensor_tensor", r=[("sg", s, hf), ("ps", 4 * s + 2 + hf)], w=[("act", fc)],
                        out=act[:, fc, hf * H:(hf + 1) * H], in0=sg[s][:, hf * H:(hf + 1) * H], in1=ps[:, 4 * s + 2 + hf, 0:H], op=ALU.mult)
            for oc in range(KC):
                wb = w2n % 2; w2n += 1
                if oc + 1 < KC:
                    load_w2(oc + 1, w2n % 2)
                elif tt + 1 < NT:
                    load_w1(0, w1n % 2)
                pb = 2 * (oc % 4)
                for fc in range(NFC):
                    for hf in range(2):
                        P.mm(ps[:, pb + hf, 0:H], wds[wb][:, fc * 128:(fc + 1) * 128], act[:, fc, hf * H:(hf + 1) * H],
                             fc == 0, fc == NFC - 1, r=[("wd", wb), ("act", fc)], w=[("ps", pb + hf)])
                ob = oc % 2
                for hf in range(2):
                    P.e("vector", "tensor_tensor", r=[("ps", pb + hf), "gbc"], w=[("yo", ob)],
                        out=yo[ob][:, hf * H:(hf + 1) * H], in0=ps[:, pb + hf, 0:H], in1=gbc[:, hf * H:(hf + 1) * H], op=ALU.mult)
                outs.append(P.d("sync", yv[:, oc, tt * TT:(tt + 1) * TT], yo[ob][:], r=[("yo", ob)], w=[("out", tt, oc)]))
        P.final_wait("sync", outs)
        P.run()
    return nc

def lay_w1(w, NFC):
    FF = w.shape[1]
    if FF < NFC * 128:
        w = np.concatenate([w, np.zeros((w.shape[0], NFC * 128 - FF), w.dtype)], 1)
    return np.ascontiguousarray(w.reshape(KC, 128, NFC, 128).transpose(2, 1, 0, 3)).reshape(NFC, 128, KC * 128)

def lay_w2(w, NFC):
    FF = w.shape[0]
    if FF < NFC * 128:
        w = np.concatenate([w, np.zeros((NFC * 128 - FF, w.shape[1]), w.dtype)], 0)
    return np.ascontiguousarray(w.reshape(NFC, 128, KC, 128).transpose(2, 1, 0, 3)).reshape(KC, 128, NFC * 128)


SEGS = ((0, 256), (256, 1152))
PIECES = ((0, 0, 256, 0), (0, 256, 384, 1), (1, 0, 384, 1), (2, 0, 384, 1))

def build_tok(kind, moe=False, final=False):
    nc = bass.Bass("TRN2", target_bir_lowering=False)
    NV = 5 if kind == "T0" else 7
    VO = 0 if kind == "T0" else 2
    xT = nc.dram_tensor("xT", [D, T], F32, kind="ExternalInput").ap()
    vec = nc.dram_tensor("vec", [128, KC * NV], F32, kind="ExternalInput").ap()
    if kind == "Ta":
        OT = nc.dram_tensor("OT", [D, T], BF16, kind="ExternalInput").ap()
        wo = nc.dram_tensor("wo", [KC, 128, KC * 128], F32, kind="ExternalInput").ap()
    if kind == "Tb":
        yP = nc.dram_tensor("yP", [NE, D, T], BF16, kind="ExternalInput").ap()
        identb = nc.dram_tensor("identb", [128, 128], BF16, kind="ExternalInput").ap()
    if moe:
        rt = nc.dram_tensor("rt", [128, KC * NE], F32, kind="ExternalInput").ap()
        ident8 = nc.dram_tensor("ident8", [NE, NE], F32, kind="ExternalInput").ap()
        gout = nc.dram_tensor("gates", [T, NE], F32, kind="ExternalOutput").ap()
    if kind != "T0":
        xo = nc.dram_tensor("xo", [D, T], F32, kind="ExternalOutput").ap()
    hT = nc.dram_tensor("hT", [D, T], F32 if final else BF16, kind="ExternalOutput").ap()
    xv = xT.rearrange("(c p) t -> p c t", p=128)
    hv = hT.rearrange("(c p) t -> p c t", p=128)
    with contextlib.ExitStack() as st:
        sb = lambda n, s, d: st.enter_context(nc.sbuf_tensor(n, s, d))
        xs = sb("xs", [128, KC, T], F32)
        vs = sb("vs", [128, KC, NV], F32)
        amod = sb("amod", [128, KC, 2], F32); atmp = sb("atmp", [128, KC], F32)
        ones = sb("ones", [128, 128], F32); epst = sb("epst", [128, 1], F32)
        sq = [sb("sq%d" % i, [128, T], F32) for i in range(2)]
        rstd = sb("rstd", [128, T], F32); sd = sb("sd", [128, T], F32)
        xn = [sb("xn%d" % i, [128, T], F32) for i in range(2)]
        hf = [sb("hf%d" % i, [128, T], F32) for i in range(2)]
        ho = [sb("ho%d" % i, [128, T], F32 if final else BF16) for i in range(2)]
        ps = st.enter_context(nc.psum_tensor("ps", [128, 8, 512], F32))
        P = Prog(nc)
        outs = []
        P.d("sync", xs[:], xv, w=["x"])
        P.d("sync", vs[:].rearrange("p c v -> p (c v)"), vec, w=["vs"])
        P.e("vector", "memset", w=["ones"], ap=ones[:], constant=1.0 / D)
        P.e("vector", "memset", w=["eps"], ap=epst[:], constant=EPS)
        for s in range(2):
            P.e("vector", "tensor_scalar", r=["vs"], w=["atmp"], out=atmp[:], in0=vs[:, :, VO + 1 + s], scalar1=1.0, scalar2=None, op0=ALU.add)
            P.e("vector", "tensor_tensor", r=["atmp", "vs"], w=["amod"], out=amod[:, :, s], in0=atmp[:], in1=vs[:, :, VO], op=ALU.mult)

        def resid(c, pb):
            for (b, lo, hi, s) in PIECES:
                P.e("vector", "scalar_tensor_tensor", r=[("ps", pb + b), "vs", ("x", c)], w=[("x", c)],
                    out=xs[:, c, b * H + lo:b * H + hi], in0=ps[:, pb + b, lo:hi], scalar=vs[:, c, s:s + 1],
                    in1=xs[:, c, b * H + lo:b * H + hi], op0=ALU.mult, op1=ALU.add)

        if kind == "Ta":
            ot = sb("ot", [128, KC, T], BF16)
            wos = [sb("wos%d" % i, [128, KC * 128], BF16) for i in range(2)]
            P.d("sync", ot[:], OT.rearrange("(c p) t -> p c t", p=128), w=["ot"])
            P.d("gpsimd", wos[0][:], wo[0], w=[("wo", 0)])
            for oc in range(KC):
                wb = oc % 2
                if oc + 1 < KC:
                    P.d("gpsimd", wos[1 - wb][:], wo[oc + 1], w=[("wo", 1 - wb)])
                pb = 3 * (oc % 2)
                for kc in range(KC):
                    for b in range(3):
                        P.mm(ps[:, pb + b, 0:H], wos[wb][:, kc * 128:(kc + 1) * 128], ot[:, kc, b * H:(b + 1) * H], kc == 0, kc == KC - 1,
                             r=[("wo", wb), "ot"], w=[("ps", pb + b)])
                resid(oc, pb)
        if kind == "Tb":
            idb = sb("idb", [128, 128], BF16)
            pt = [sb("pt%d" % i, [128, NE, T], BF16) for i in range(2)]
            ypv = yP.rearrange("e (c p) t -> p c e t", p=128)
            P.d("sync", idb[:], identb, w=["idb"])
            P.d("sync", pt[0][:], ypv[:, 0], w=[("pt", 0)])
            for c in range(KC):
                tb = c % 2
                if c + 1 < KC:
                    P.d("sync", pt[1 - tb][:], ypv[:, c + 1], w=[("pt", 1 - tb)])
                pb = 3 * (c % 2)
                for e_ in range(NE):
                    for b in range(3):
                        P.mm(ps[:, pb + b, 0:H], idb[:], pt[tb][:, e_, b * H:(b + 1) * H], e_ == 0, e_ == NE - 1,
                             r=["idb", ("pt", tb)], w=[("ps", pb + b)])
                resid(c, pb)
        xtoks = ["x"] + [("x", c) for c in range(KC)]
        if kind != "T0":
            outs.append(P.d("sync", xo.rearrange("(c p) t -> p c t", p=128), xs[:], r=xtoks, w=["xo"]))
        for c in range(KC):
            P.e("scalar", "activation", r=["x", ("x", c)], w=[("sq", c % 2)], out=sq[c % 2][:], in_=xs[:, c, :], func=AF.Square)
            for b in range(3):
                P.mm(ps[:, b, 0:H], ones[:], sq[c % 2][:, b * H:(b + 1) * H], c == 0, c == KC - 1, r=["ones", ("sq", c % 2)], w=[("ps", b)])
        for b in range(3):
            P.e("scalar", "activation", r=[("ps", b), "eps"], w=["sd"], out=sd[:, b * H:(b + 1) * H], in_=ps[:, b, 0:H], func=AF.Sqrt, bias=epst[:, 0:1], scale=1.0)
        P.e("vector", "reciprocal", r=["sd"], w=["rstd"], out=rstd[:], in_=sd[:])
        if moe:
            rts = sb("rts", [128, KC, NE], F32); id8 = sb("id8", [NE, NE], F32)
            lg = sb("lg", [NE, T], F32)
            P.d("sync", rts[:].rearrange("p c v -> p (c v)"), rt, w=["rts"])
            P.d("sync", id8[:], ident8, w=["id8"])
        for c in range(KC):
            k = c % 2
            P.e("vector", "tensor_tensor", r=["x", ("x", c), "rstd"], w=[("xn", k)], out=xn[k][:], in0=xs[:, c, :], in1=rstd[:], op=ALU.mult)
            for s, (lo, hi) in enumerate(SEGS):
                P.e("vector", "tensor_scalar", r=[("xn", k), "amod", "vs"], w=[("hf", k)], out=hf[k][:, lo:hi], in0=xn[k][:, lo:hi],
                    scalar1=amod[:, c, s:s + 1], scalar2=vs[:, c, VO + 3 + s:VO + 4 + s], op0=ALU.mult, op1=ALU.add)
            if moe:
                for b in range(3):
                    P.mm(ps[0:NE, 3 + b, 0:H], rts[:, c, :], hf[k][:, b * H:(b + 1) * H], c == 0, c == KC - 1, r=["rts", ("hf", k)], w=[("ps", 3 + b)])
            P.e("scalar", "copy", r=[("hf", k)], w=[("ho", k)], out=ho[k][:], in_=hf[k][:])
            outs.append(P.d("sync", hv[:, c, :], ho[k][:], r=[("ho", k)], w=[("hout", c)]))
        if moe:
            NTL = T // 128
            L = sb("L", [128, NTL, NE], F32); m1 = sb("m1", [128, NTL], F32); nm1 = sb("nm1", [128, NTL], F32)
            eq = sb("eq", [128, NTL, NE], F32); L2 = sb("L2", [128, NTL, NE], F32); m2 = sb("m2", [128, NTL], F32)
            sel = sb("sel", [128, NTL, NE], F32); ex = sb("ex", [128, NTL, NE], F32); sm = sb("sm", [128, NTL], F32)
            gt = sb("gt", [128, NTL, NE], F32)
            for b in range(3):
                P.e("vector", "tensor_copy", r=[("ps", 3 + b)], w=["lg"], out=lg[:, b * H:(b + 1) * H], in_=ps[0:NE, 3 + b, 0:H])
            for t in range(NTL):
                P.mm(ps[:, 6, t * NE:(t + 1) * NE], lg[:, t * 128:(t + 1) * 128], id8[:], True, True, r=["lg", "id8"], w=[("ps", 6)])
            P.e("vector", "tensor_copy", r=[("ps", 6)], w=["L"], out=L[:].rearrange("p t e -> p (t e)"), in_=ps[:, 6, 0:NTL * NE])
            bc = lambda a: a[:].unsqueeze(2).to_broadcast([128, NTL, NE])
            P.e("vector", "tensor_reduce", r=["L"], w=["m1"], out=m1[:], in_=L[:], axis=AX.X, op=ALU.max)
            P.e("vector", "tensor_tensor", r=["L", "m1"], w=["eq"], out=eq[:], in0=L[:], in1=bc(m1), op=ALU.is_equal)
            P.e("vector", "scalar_tensor_tensor", r=["eq", "L"], w=["L2"], out=L2[:], in0=eq[:], scalar=-1e30, in1=L[:], op0=ALU.mult, op1=ALU.add)
            P.e("vector", "tensor_reduce", r=["L2"], w=["m2"], out=m2[:], in_=L2[:], axis=AX.X, op=ALU.max)
            P.e("vector", "tensor_tensor", r=["L", "m2"], w=["sel"], out=sel[:], in0=L[:], in1=bc(m2), op=ALU.is_ge)
            P.e("vector", "tensor_tensor", r=["L", "m1"], w=["ex"], out=ex[:], in0=L[:], in1=bc(m1), op=ALU.subtract)
            P.e("scalar", "activation", r=["ex"], w=["ex2"], out=eq[:], in_=ex[:], func=AF.Exp)
            P.e("vector", "tensor_tensor", r=["ex2", "sel"], w=["pp"], out=L2[:], in0=eq[:], in1=sel[:], op=ALU.mult)
            P.e("vector", "tensor_reduce", r=["pp"], w=["sm"], out=sm[:], in_=L2[:], axis=AX.X, op=ALU.add)
            P.e("vector", "reciprocal", r=["sm"], w=["rs"], out=m2[:], in_=sm[:])
            P.e("vector", "tensor_tensor", r=["pp", "rs"], w=["gt"], out=gt[:], in0=L2[:], in1=bc(m2), op=ALU.mult)
            outs.append(P.d("sync", gout.rearrange("(t p) e -> p t e", p=128), gt[:], r=["gt"], w=["gout"]))
        P.final_wait("sync", outs)
        P.run()
    return nc

def lay_vec(cols):
    v = np.stack([np.asarray(c, np.float32) for c in cols], 1)
    return np.ascontiguousarray(v.reshape(KC, 128, -1).transpose(1, 0, 2)).reshape(128, -1)


NEG = -30000.0

def nat_type(t):
    return 0 if t == 0 else 1 if t == 1 else 2 if t <= 13 else 3 if t == 14 else 4

def build_nat(NB=4):
    nc = bass.Bass("TRN2", target_bir_lowering=False)
    NTOK = NB * SB
    hT = nc.dram_tensor("hT", [D, NTOK], BF16, kind="ExternalInput").ap()
    wqk = nc.dram_tensor("wqk", [4, 128, KC * 128], F32, kind="ExternalInput").ap()
    wv = nc.dram_tensor("wv", [128, KC * 256], F32, kind="ExternalInput").ap()
    bias = nc.dram_tensor("bias", [128, 2 * 5 * 640], F32, kind="ExternalInput").ap()
    identb = nc.dram_tensor("identb", [128, 128], BF16, kind="ExternalInput").ap()
    O = nc.dram_tensor("O", [NTOK, 256], BF16, kind="ExternalOutput").ap()
    hv = hT.rearrange("(c p) t -> p c t", p=128)
    with contextlib.ExitStack() as st:
        sb = lambda n, s, d: st.enter_context(nc.sbuf_tensor(n, s, d))
        hs = sb("hs", [128, KC, SB], BF16)
        wqks = sb("wqks", [128, 4, KC * 128], BF16)
        wvs = sb("wvs", [128, KC * 256], BF16)
        bs = sb("bs", [128, 2, 5, 640], F32)
        idb = sb("idb", [128, 128], BF16)
        qk = sb("qk", [128, 4, SB], BF16)
        va = sb("va", [128, NTB, 2, DH + 1], BF16)
        ss = [sb("ss%d" % i, [128, 896], F32) for i in range(2)]
        pp = [sb("pp%d" % i, [128, 896], BF16) for i in range(2)]
        pT = [sb("pT%d" % i, [128, 7, 128], BF16) for i in range(2)]
        mx = sb("mx", [128, 2], F32); nmx = sb("nmx", [128, 2], F32); rc = sb("rc", [128, 2], F32)
        oo = [sb("oo%d" % i, [128, NTB, 256], BF16) for i in range(2)]
        ps = st.enter_context(nc.psum_tensor("ps", [128, 6, 512], F32))
        pst = [st.enter_context(nc.psum_tensor("pst%d" % i, [128, 1024], BF16)) for i in range(2)]
        P = Prog(nc)
        outs = []
        for i in range(4):
            P.d("gpsimd", wqks[:, i, :], wqk[i], w=[("wqk", i)])
        P.d("gpsimd", wvs[:], wv, w=["wv"])
        P.d("sync", bs[:].rearrange("p a b k -> p (a b k)"), bias, w=["bias"])
        P.d("sync", idb[:], identb, w=["idb"])
        P.e("vector", "memset", w=["va1"], ap=va[:, :, :, DH:DH + 1], constant=1.0)
        scale = float(DH) ** -0.5
        for b in range(NB):
            P.d("sync", hs[:], hv[:, :, b * SB:(b + 1) * SB], w=["hs"])
            n = 0
            for blk in range(4):
                for tg in range(6):
                    pb = n % 2; n += 1
                    for kc in range(KC):
                        P.mm(ps[:, pb, 0:384], wqks[:, blk, kc * 128:(kc + 1) * 128], hs[:, kc, tg * 384:(tg + 1) * 384], kc == 0, kc == KC - 1,
                             r=[("wqk", blk), "hs"], w=[("ps", pb)])
                    if blk < 2:
                        P.e("scalar", "activation", r=[("ps", pb)], w=[("qk", blk)], out=qk[:, blk, tg * 384:(tg + 1) * 384], in_=ps[:, pb, 0:384], func=AF.Copy, scale=scale)
                    else:
                        P.e("vector", "tensor_copy", r=[("ps", pb)], w=[("qk", blk)], out=qk[:, blk, tg * 384:(tg + 1) * 384], in_=ps[:, pb, 0:384])
            for tl in range(NTB):
                pb = n % 2; n += 1
                for kc in range(KC):
                    P.mm(ps[:, pb, 0:256], hs[:, kc, tl * 128:(tl + 1) * 128], wvs[:, kc * 256:(kc + 1) * 256], kc == 0, kc == KC - 1,
                         r=["wv", "hs"], w=[("ps", pb)])
                P.e("vector", "tensor_copy", r=[("ps", pb)], w=["va"], out=va[:, tl, :, 0:DH], in_=ps[:, pb, 0:256].rearrange("p (j d) -> p j d", j=2))
            ob = b % 2
            for qt in range(NTB):
                lat = qt >= 2
                if lat:
                    t = qt - 2
                    lt0 = min(max(t - 2, 0), 11)
                    k0 = (2 + lt0) * 128
                    ktiles = [2 + lt0 + i for i in range(5)] + [0, 1]
                    ty = nat_type(t); W = 896
                else:
                    ktiles = [0, 1]; W = 256
                NK = len(ktiles)
                qs = slice(qt * 128, (qt + 1) * 128)
                for j in range(2):
                    bA, bB = 2 + 2 * j, 3 + 2 * j
                    rq = [("qk", j), ("qk", 2 + j)]
                    if lat:
                        P.mm(ps[:, bA, 0:512], qk[:, j, qs], qk[:, 2 + j, k0:k0 + 512], True, True, r=rq, w=[("ps", bA)])
                        P.mm(ps[:, bB, 0:128], qk[:, j, qs], qk[:, 2 + j, k0 + 512:k0 + 640], True, True, r=rq, w=[("ps", bB)])
                    P.mm(ps[:, bB, 128:384], qk[:, j, qs], qk[:, 2 + j, 0:256], True, True, r=rq, w=[("ps", bB)])
                for j in range(2):
                    bA, bB = 2 + 2 * j, 3 + 2 * j
                    if lat:
                        P.e("vector", "tensor_tensor", r=[("ps", bA), "bias"], w=[("ss", j)], out=ss[j][:, 0:512], in0=ps[:, bA, 0:512], in1=bs[:, j, ty, 0:512], op=ALU.add)
                        P.e("vector", "tensor_tensor", r=[("ps", bB), "bias"], w=[("ss", j)], out=ss[j][:, 512:640], in0=ps[:, bB, 0:128], in1=bs[:, j, ty, 512:640], op=ALU.add)
                        P.e("scalar", "copy", r=[("ps", bB)], w=[("ss", j)], out=ss[j][:, 640:896], in_=ps[:, bB, 128:384])
                    else:
                        P.e("scalar", "copy", r=[("ps", bB)], w=[("ss", j)], out=ss[j][:, 0:256], in_=ps[:, bB, 128:384])
                for j in range(2):
                    P.e("vector", "tensor_reduce", r=[("ss", j)], w=[("mx", j)], out=mx[:, j:j + 1], in_=ss[j][:, 0:W], axis=AX.X, op=ALU.max)
                    P.e("vector", "tensor_scalar", r=[("mx", j)], w=[("nmx", j)], out=nmx[:, j:j + 1], in0=mx[:, j:j + 1], scalar1=-1.0, scalar2=None, op0=ALU.mult)
                for j in range(2):
                    P.e("scalar", "activation", r=[("ss", j), ("nmx", j)], w=[("pp", j)], out=pp[j][:, 0:W], in_=ss[j][:, 0:W], func=AF.Exp, bias=nmx[:, j:j + 1], scale=1.0)
                for j in range(2):
                    for i in range(NK):
                        P.op("tensor", lambda en, a=(pst[j][:, i * 128:(i + 1) * 128], pp[j][:, i * 128:(i + 1) * 128], idb[:]): en.transpose(a[0], a[1], a[2]),
                             reads=[("pp", j), "idb"], writes=[("pst", j)])
                for j in range(2):
                    eng = "scalar" if j == 0 else "vector"
                    P.e(eng, "copy" if j == 0 else "tensor_copy", r=[("pst", j)], w=[("pT", j)], out=pT[j][:, 0:NK, :].rearrange("p a b -> p (a b)"), in_=pst[j][:, 0:NK * 128])
                for j in range(2):
                    for i, kt in enumerate(ktiles):
                        P.mm(ps[:, j, 0:DH + 1], pT[j][:, i, :], va[:, kt, j, :], i == 0, i == NK - 1, r=[("pT", j), "va", "va1"], w=[("ps", j)])
                for j in range(2):
                    P.e("vector", "reciprocal", r=[("ps", j)], w=[("rc", j)], out=rc[:, j:j + 1], in_=ps[:, j, DH:DH + 1])
                    P.e("vector", "tensor_scalar", r=[("ps", j), ("rc", j)], w=[("oo", ob)], out=oo[ob][:, qt, j * DH:(j + 1) * DH], in0=ps[:, j, 0:DH],
                        scalar1=rc[:, j:j + 1], scalar2=None, op0=ALU.mult)
            outs.append(P.d("sync", O[b * SB:(b + 1) * SB, :].rearrange("(t p) f -> p t f", p=128), oo[ob][:], r=[("oo", ob)], w=[("O", b)]))
        P.final_wait("sync", outs)
        P.run()
    return nc

def nat_bias_tiles(rpb2):
    out = np.full((128, 2, 5, 640), NEG, np.float32)
    qq = np.arange(128); kk = np.arange(640)
    for ty, t in enumerate((0, 1, 5, 14, 15)):
        base = 2 * min(max(t - 2, 0), 11)
        r = 2 * t + qq // 64; qc = qq % 64
        kr = base + kk // 64; kc_ = kk % 64
        rs = np.clip(r - 4, 0, 24); cs = np.clip(qc - 8, 0, 48)
        ok = (kr[None, :] >= rs[:, None]) & (kr[None, :] < rs[:, None] + 8) & (kc_[None, :] >= cs[:, None]) & (kc_[None, :] < cs[:, None] + 16)
        dr = np.clip(kr[None, :] - r[:, None] + 7, 0, 14); dc = np.clip(kc_[None, :] - qc[:, None] + 15, 0, 30)
        for j in range(2):
            out[:, j, ty, :] = np.where(ok, rpb2[j][dr, dc], NEG)
    return out.reshape(128, -1)


def build_gla(NB=2, dbg=9, sub=9):
    nc = bass.Bass("TRN2", target_bir_lowering=False)
    NT = NB * NTB
    hTt = nc.dram_tensor("hTt", [NT, 128, KC * 128], BF16, kind="ExternalInput").ap()
    wfm = nc.dram_tensor("wfm", [8, 128, KC * 128], F32, kind="ExternalInput").ap()
    wz = nc.dram_tensor("wz", [128, KC * 64], F32, kind="ExternalInput").ap()
    wvr = nc.dram_tensor("wvr", [128, KC * 1024], F32, kind="ExternalInput").ap()
    wgate = nc.dram_tensor("wgate", [16, 512], F32, kind="ExternalInput").ap()
    bgate = nc.dram_tensor("bgate", [128, 512], F32, kind="ExternalInput").ap()
    rope = nc.dram_tensor("rope", [128, 2 * 96], F32, kind="ExternalInput").ap()
    ng = nc.dram_tensor("ng", [128, DV], F32, kind="ExternalInput").ap()
    tri = nc.dram_tensor("tri", [128, 4 * 128], F32, kind="ExternalInput").ap()
    identf = nc.dram_tensor("identf", [128, 128], F32, kind="ExternalInput").ap()
    Og = nc.dram_tensor("Og", [NB * SB, DV], BF16, kind="ExternalOutput").ap()
    with contextlib.ExitStack() as st:
        sb = lambda n, s, d: st.enter_context(nc.sbuf_tensor(n, s, d))
        hs = [sb("hs0", [128, KC, 128], BF16)] * 2
        wfms = sb("wfms", [128, 8, KC * 128], BF16)
        wzs = sb("wzs", [128, KC * 64], BF16)
        wvrs = sb("wvrs", [128, KC * 1024], BF16)
        wgs = sb("wgs", [16, 512], F32); bgs = sb("bgs", [128, 512], F32)
        rp = sb("rp", [128, 2, 96], F32); ngs = sb("ngs", [128, DV], F32)
        tr = sb("tr", [128, 4, 128], F32); idf = sb("idf", [128, 128], F32)
        vs = sb("vs", [128, NTB, DV], BF16); rs = sb("rs", [128, NTB, DV], BF16)
        oacc = sb("oacc", [128, NTB, DV], F32)
        qpf = [sb("qpf%d" % i, [128, 2, 128], BF16) for i in range(2)]
        kpf = [sb("kpf%d" % i, [128, 2, 128], BF16) for i in range(2)]
        ktf = [sb("ktf%d" % i, [128, 256], BF16) for i in range(2)]
        qpb = sb("qpb", [128, NTB, 2, 128], BF16); kpb = sb("kpb", [128, NTB, 2, 128], BF16); ktb = sb("ktb", [128, NTB, 256], BF16)
        el = sb("el", [128, 2, NTB, 2], F32)
        S = sb("S", [128, 2, 2, DV], F32); Sb = sb("Sb", [128, 2, 2, DV], BF16)
        zsb = sb("zsb", [16, 2, 128], F32)
        qr = sb("qr", [128, 2, 128], F32); kr = sb("kr", [128, 2, 128], F32)
        t1 = sb("t1", [128, 128], F32); t2 = sb("t2", [128, 128], F32)
        scr = sb("scr", [128, 1024], F32); zp = scr[:, 0:256]; za = scr[:, 256:512]; ze = scr[:, 512:768]; gl = scr[:, 768:1024]
        Ep = sb("Ep", [128, 2, 128], F32); Em = sb("Em", [128, 2, 128], F32); kpf32 = sb("kpf32", [128, 2, 128], F32)
        at = sb("at", [128, 128], BF16)
        sqt = scr[:, 0:512]; ssum = sb("ssum", [128, 1], F32); rstd = sb("rstd", [128, 1], F32); t5 = scr[:, 512:1024]
        epst = sb("epst", [128, 1], F32)
        ps = st.enter_context(nc.psum_tensor("ps", [128, 8, 512], F32))
        P = Prog(nc)
        outs = []
        for i in range(8):
            P.d("gpsimd", wfms[:, i, :], wfm[i], w=["wfm"])
        P.d("gpsimd", wzs[:], wz, w=["wz"])
        P.d("gpsimd", wvrs[:], wvr, w=["wvr"])
        for (dst, src, tok) in ((wgs[:], wgate, "wg"), (bgs[:], bgate, "bg"), (rp[:].rearrange("p a b -> p (a b)"), rope, "rp"), (ngs[:], ng, "ng"),
                                (tr[:].rearrange("p a b -> p (a b)"), tri, "tr"), (idf[:], identf, "idf")):
            P.d("sync", dst, src, w=[tok])
        P.e("vector", "memset", w=["eps"], ap=epst[:], constant=EPS)
        P.d("sync", hs[0][:].rearrange("p c t -> p (c t)"), hTt[0], w=[("hs", 0)])
        HSINGLE = True

        def scan_step(d, tl, qp, kp, kt):
            rq = [("qp", d, tl), ("kp", d, tl)]
            for ch in range(2):
                P.mm(ps[:, 7, 0:128], kp[:, ch, :], qp[:, ch, :], ch == 0, ch == 1, r=rq, w=[("ps", 7)])
            P.e("vector", "tensor_tensor", r=[("ps", 7), "tr"], w=["at"], out=at[:], in0=ps[:, 7, 0:128], in1=tr[:, 2 + d, :], op=ALU.mult)
            if sub < 2: return
            P.mm(ps[:, 6, :], at[:], vs[:, tl, :], True, False, r=["at", ("v", tl)], w=[("ps", 6)])
            for ch in range(2):
                P.mm(ps[:, 6, :], qp[:, ch, :], Sb[:, d, ch, :], False, ch == 1, r=rq + [("Sb", d)], w=[("ps", 6)])
            if d == 0:
                P.e("scalar", "copy", r=[("ps", 6)], w=[("o", tl)], out=oacc[:, tl, :], in_=ps[:, 6, :])
            else:
                P.e("vector", "tensor_tensor", r=[("ps", 6), ("o", tl)], w=[("o", tl)], out=oacc[:, tl, :], in0=ps[:, 6, :], in1=oacc[:, tl, :], op=ALU.add)
            if sub < 3: return
            for ch in range(2):
                P.mm(ps[:, 7, :], kt[:, ch * 128:(ch + 1) * 128], vs[:, tl, :], True, True, r=[("kt", d, tl), ("v", tl)], w=[("ps", 7)])
                e_ = el[:, d, tl, ch:ch + 1]
                P.e("vector", "tensor_scalar", r=[("S", d), ("el", d, tl)], w=[("S", d)], out=S[:, d, ch, :], in0=S[:, d, ch, :], scalar1=e_, scalar2=None, op0=ALU.mult)
                P.e("vector", "scalar_tensor_tensor", r=[("ps", 7), ("S", d), ("el", d, tl)], w=[("S", d)], out=S[:, d, ch, :], in0=ps[:, 7, :], scalar=e_,
                    in1=S[:, d, ch, :], op0=ALU.mult, op1=ALU.add)
                P.e("scalar", "copy", r=[("S", d)], w=[("Sb", d)], out=Sb[:, d, ch, :], in_=S[:, d, ch, :])

        g = 0
        for b in range(NB):
            P.e("vector", "memset", w=[("S", 0), ("S", 1)], ap=S[:].rearrange("p a b c -> p (a b c)"), constant=0.0)
            P.e("vector", "memset", w=[("Sb", 0), ("Sb", 1)], ap=Sb[:].rearrange("p a b c -> p (a b c)"), constant=0.0)
            for tl in range(NTB):
                hb = 0
                if g > 0:
                    P.d("sync", hs[0][:].rearrange("p c t -> p (c t)"), hTt[g], w=[("hs", 0)])
                g += 1
                lat = tl >= 2; t = tl - 2
                for blk in range(8):
                    for kc in range(KC):
                        P.mm(ps[:, blk // 4, (blk % 4) * 128:(blk % 4 + 1) * 128], wfms[:, blk, kc * 128:(kc + 1) * 128], hs[hb][:, kc, :], kc == 0, kc == KC - 1,
                             r=["wfm", ("hs", hb)], w=[("ps", "pq", blk)])
                for d in range(2):
                    for kc in range(KC):
                        P.mm(ps[0:16, 2, 0:128] if d == 0 else ps[0:16, 5, 384:512],
                             wzs[:, kc * 64 + 32 * d:kc * 64 + 32 * d + 16], hs[hb][:, kc, :], kc == 0, kc == KC - 1, r=["wz", ("hs", hb)], w=[("ps", 2, "z") if d == 0 else ("ps", 5, "b")])
                for (bank, off) in ((3, 0), (4, 512)):
                    for kc in range(KC):
                        P.mm(ps[:, bank, :], hs[hb][:, kc, :], wvrs[:, kc * 1024 + off:kc * 1024 + off + 512], kc == 0, kc == KC - 1, r=["wvr", ("hs", hb)], w=[("ps", bank)])
                P.e("scalar", "copy", r=[("ps", 3)], w=[("v", tl)], out=vs[:, tl, :], in_=ps[:, 3, :])
                P.e("scalar", "activation", r=[("ps", 4)], w=[("r", tl)], out=rs[:, tl, :], in_=ps[:, 4, :], func=AF.Silu)
                P.e("vector", "tensor_copy", r=[("ps", 2, "z")], w=["zsb"], out=zsb[:, 0, :], in_=ps[0:16, 2, 0:128])
                P.e("vector", "tensor_copy", r=[("ps", 5, "b")], w=["zsb"], out=zsb[:, 1, :], in_=ps[0:16, 5, 384:512])
                if dbg < 2: continue
                for (dst, tok, b0) in ((qr, "qr", 0), (kr, "kr", 4)):
                    for ch in range(2):
                        src = ps[:, (b0 + ch) // 4, ((b0 + ch) % 4) * 128:((b0 + ch) % 4 + 1) * 128]
                        ssw = ps[:, (b0 + 2 + ch) // 4, ((b0 + 2 + ch) % 4) * 128:((b0 + 2 + ch) % 4 + 1) * 128]
                        rd = [("ps", "pq", b0 + ch), ("ps", "pq", b0 + 2 + ch)]
                        if lat:
                            v3 = lambda a: a.rearrange("p (a b) -> p a b", a=2)
                            if ch == 0:
                                cb = rp[:, 0, 2 * t:2 * t + 2].unsqueeze(2).to_broadcast([128, 2, 64]); sbb = rp[:, 1, 2 * t:2 * t + 2].unsqueeze(2).to_broadcast([128, 2, 64])
                            else:
                                cb = rp[:, 0, 32:96].unsqueeze(1).to_broadcast([128, 2, 64]); sbb = rp[:, 1, 32:96].unsqueeze(1).to_broadcast([128, 2, 64])
                            P.e("vector", "tensor_tensor", r=rd + ["rp"], w=["t1"], out=v3(t1[:]), in0=v3(src), in1=cb, op=ALU.mult)
                            P.e("vector", "tensor_tensor", r=rd + ["rp"], w=["t2"], out=v3(t2[:]), in0=v3(ssw), in1=sbb, op=ALU.mult)
                            P.e("vector", "tensor_tensor", r=["t1", "t2"], w=[tok], out=dst[:, ch, :], in0=t1[:], in1=t2[:], op=ALU.add)
                        else:
                            P.e("vector", "tensor_copy", r=rd, w=[tok], out=dst[:, ch, :], in_=src)
                if dbg < 3: continue
                for d in range(2):
                    P.mm(ps[:, 5, 0:256], zsb[:, d, :], wgs[:, d * 256:(d + 1) * 256], True, True, r=["zsb", "wg"], w=[("ps", 5, "z")])
                    P.e("vector", "tensor_tensor", r=[("ps", 5, "z"), "bg"], w=["scr"], out=zp, in0=ps[:, 5, 0:256], in1=bgs[:, d * 256:(d + 1) * 256], op=ALU.add)
                    P.e("vector", "scalar_tensor_tensor", r=["scr"], w=["scr"], out=za, in0=zp, scalar=-1.0, in1=zp, op0=ALU.mult, op1=ALU.max)
                    P.e("scalar", "activation", r=["scr"], w=["scr"], out=ze, in_=za, func=AF.Exp, scale=-1.0)
                    P.e("scalar", "activation", r=["scr"], w=["scr"], out=za, in_=ze, func=AF.Ln, bias=1.0, scale=1.0)
                    P.e("vector", "tensor_scalar", r=["scr"], w=["scr"], out=ze, in0=zp, scalar1=0.0, scalar2=None, op0=ALU.min)
                    P.e("vector", "tensor_tensor", r=["scr", "scr"], w=["scr"], out=gl, in0=ze, in1=za, op=ALU.subtract)
                    for ch in range(2):
                        P.mm(ps[:, 5, 256 + ch * 128:256 + (ch + 1) * 128], gl[:, ch * 128:(ch + 1) * 128], tr[:, d, :], True, True, r=["scr", "tr"], w=[("ps", 5, "b")])
                    P.e("scalar", "activation", r=[("ps", 5, "b")], w=["Ep"], out=Ep[:].rearrange("p a b -> p (a b)"), in_=ps[:, 5, 256:512], func=AF.Exp)
                    P.e("scalar", "activation", r=[("ps", 5, "b")], w=["Em"], out=Em[:].rearrange("p a b -> p (a b)"), in_=ps[:, 5, 256:512], func=AF.Exp, scale=-1.0)
                    col = 127 if d == 0 else 0
                    P.e("vector", "tensor_copy", r=["Ep"], w=[("el", d, tl)], out=el[:, d, tl, :], in_=Ep[:, :, col])
                    qp = qpf[tl % 2] if d == 0 else qpb[:, tl]
                    kp = kpf[tl % 2] if d == 0 else kpb[:, tl]
                    kt = ktf[tl % 2] if d == 0 else ktb[:, tl]
                    qpv = qp[:] if d == 0 else qp
                    kpv = kp[:] if d == 0 else kp
                    ktv = kt[:] if d == 0 else kt
                    P.e("vector", "scalar_tensor_tensor", r=["qr", "Ep"], w=[("qp", d, tl)], out=qpv, in0=qr[:], scalar=1.0 / 16.0, in1=Ep[:], op0=ALU.mult, op1=ALU.mult)
                    P.e("vector", "tensor_tensor", r=["kr", "Em"], w=["kpf32"], out=kpf32[:], in0=kr[:], in1=Em[:], op=ALU.mult)
                    P.e("scalar", "copy", r=["kpf32"], w=[("kp", d, tl)], out=kpv, in_=kpf32[:])
                    for ch in range(2):
                        P.mm(ps[:, 2, 256 + ch * 128:256 + (ch + 1) * 128], kpf32[:, ch, :], idf[:], True, True, r=["kpf32", "idf"], w=[("ps", 2, "kt")])
                    P.e("vector", "tensor_copy", r=[("ps", 2, "kt")], w=[("kt", d, tl)], out=ktv, in_=ps[:, 2, 256:512])
                    if d == 0 and dbg >= 4:
                        scan_step(0, tl, qpv, kpv, ktv)
            for tl in ([1, 0] + list(range(NTB - 1, 1, -1)) if dbg >= 5 else []):
                scan_step(1, tl, qpb[:, tl], kpb[:, tl], ktb[:, tl])
            for tl in (range(NTB) if dbg >= 6 else []):
                P.e("scalar", "activation", r=[("o", tl)], w=["scr"], out=sqt, in_=oacc[:, tl, :], func=AF.Square)
                P.e("vector", "tensor_reduce", r=["scr"], w=["ssum"], out=ssum[:], in_=sqt, axis=AX.X, op=ALU.add)
                P.e("scalar", "activation", r=["ssum", "eps"], w=["rstd0"], out=rstd[:], in_=ssum[:], func=AF.Sqrt, bias=epst[:, 0:1], scale=1.0 / DV)
                P.e("vector", "reciprocal", r=["rstd0"], w=["rstd"], out=ssum[:], in_=rstd[:])
                P.e("vector", "scalar_tensor_tensor", r=[("o", tl), "rstd", "ng"], w=["scr"], out=t5, in0=oacc[:, tl, :], scalar=ssum[:, 0:1], in1=ngs[:], op0=ALU.mult, op1=ALU.mult)
                P.e("vector", "tensor_tensor", r=["scr", ("r", tl)], w=[("r", tl)], out=rs[:, tl, :], in0=t5, in1=rs[:, tl, :], op=ALU.mult)
            outs.append(P.d("sync", Og[b * SB:(b + 1) * SB, :].rearrange("(t p) f -> p t f", p=128), rs[:], r=[("r", tl) for tl in range(NTB)], w=[("Og", b)]))
        P.final_wait("sync", outs)
        P.run()
    return nc

def gla_consts():
    half = 128
    freqs = 10000.0 ** (-np.arange(0, half, 2, dtype=np.float32) / half)
    fr = np.concatenate([freqs, freqs])
    pos = np.concatenate([np.arange(32), np.arange(64)]).astype(np.float32)
    ang = fr[:, None] * pos[None, :]
    sgn = np.where(np.arange(128) < 64, -1.0, 1.0)[:, None]
    rope = np.concatenate([np.cos(ang), sgn * np.sin(ang)], 1).astype(np.float32)
    j = np.arange(128)[:, None]; i = np.arange(128)[None, :]
    U = (j <= i).astype(np.float32); L = (j >= i).astype(np.float32)
    tri = np.concatenate([U / 16.0, L / 16.0, U, L], 1).astype(np.float32)
    return rope, tri, np.eye(128, dtype=np.float32)


def build_ada():
    nc = bass.Bass("TRN2", target_bir_lowering=False)
    cT = nc.dram_tensor("cT", [128, KC * NR], F32, kind="ExternalInput").ap()
    w = nc.dram_tensor("w", [NL * 4, 128, 4 * NCOL], F32, kind="ExternalInput").ap()
    bia = nc.dram_tensor("bia", [1, NL * NCOL], F32, kind="ExternalInput").ap()
    m = nc.dram_tensor("m", [NR, NL * NCOL], F32, kind="ExternalOutput").ap()
    with contextlib.ExitStack() as st:
        sb = lambda n, s, d: st.enter_context(nc.sbuf_tensor(n, s, d))
        cs = sb("cs", [128, KC, NR], F32); sc = sb("sc", [128, KC, NR], F32)
        ws = [sb("ws%d" % i, [128, 4, NCOL], F32) for i in range(2)]
        bs = sb("bs", [1, NL * NCOL], F32); ones = sb("ones", [1, NR], F32)
        mo = sb("mo", [NR, NL * NCOL], F32)
        ps = st.enter_context(nc.psum_tensor("ps", [128, 8, 512], F32))
        P = Prog(nc)
        P.d("sync", cs[:].rearrange("p c r -> p (c r)"), cT, w=["cs"])
        P.d("sync", bs[:], bia, w=["bs"])
        P.e("vector", "memset", w=["ones"], ap=ones[:], constant=1.0)
        P.e("scalar", "activation", r=["cs"], w=["sc"], out=sc[:].rearrange("p c r -> p (c r)"), in_=cs[:].rearrange("p c r -> p (c r)"), func=AF.Silu)
        P.d("sync", ws[0][:].rearrange("p k c -> p (k c)"), w[0], w=[("ws", 0)])
        n = 0
        for l in range(NL):
            pb = 3 * (l % 2)
            for kg in range(4):
                b = n % 2; n += 1
                if n < NL * 4:
                    P.d("sync", ws[1 - b][:].rearrange("p k c -> p (k c)"), w[n], w=[("ws", 1 - b)])
                for k4 in range(4):
                    kc = kg * 4 + k4
                    for j in range(3):
                        P.mm(ps[0:NR, pb + j, :], sc[:, kc, :], ws[b][:, k4, j * 512:(j + 1) * 512], kc == 0, False, r=["sc", ("ws", b)], w=[("ps", pb + j)])
            for j in range(3):
                P.mm(ps[0:NR, pb + j, :], ones[:], bs[:, l * NCOL + j * 512:l * NCOL + (j + 1) * 512], False, True, r=["ones", "bs"], w=[("ps", pb + j)])
                P.e("vector", "tensor_copy", r=[("ps", pb + j)], w=["mo"], out=mo[:, l * NCOL + j * 512:l * NCOL + (j + 1) * 512], in_=ps[0:NR, pb + j, :])
        o = P.d("sync", m, mo[:], r=["mo"], w=["m"])
        P.final_wait("sync", [o])
        P.run()
    return nc

def lay_ada(ada_w, ada_b, c, c_ctx, core):
    cols = slice(core * NCOL, (core + 1) * NCOL)
    w = ada_w[:, :, cols]
    w = np.ascontiguousarray(w.reshape(NL, 4, 4, 128, NCOL).transpose(0, 1, 3, 2, 4)).reshape(NL * 4, 128, 4 * NCOL)
    cc = np.concatenate([c, c_ctx[None]], 0)
    cT = np.ascontiguousarray(cc.T.reshape(KC, 128, NR).transpose(1, 0, 2)).reshape(128, KC * NR)
    return {"cT": cT, "w": w, "bia": np.ascontiguousarray(ada_b[:, cols]).reshape(1, NL * NCOL)}


NCORES = 8
NTOK = 9216
_PROGS = {}


def _prog(key, fn):
    if key not in _PROGS:
        _PROGS[key] = fn()
    return _PROGS[key]


def _run(nc, in_maps):
    res = run_bass_kernel_spmd(nc, in_maps, core_ids=list(range(NCORES)))
    return res.results


def _bf(a):
    return np.ascontiguousarray(a)


def gla_inputs(hT_bf, wq, wk, wv, wr, wzf, wzb, wgf, wgb, bgf, bgb, ngv):
    NT = hT_bf.shape[1] // 128
    hTt = np.ascontiguousarray(hT_bf.reshape(KC, 128, NT, 128).transpose(2, 1, 0, 3)).reshape(NT, 128, KC * 128)
    sw = np.concatenate([np.arange(64, 128), np.arange(0, 64)])
    perm = np.concatenate([sw, 128 + sw])
    blocks = np.concatenate([wq, wq[:, perm], wk, wk[:, perm]], 1)
    wfm = lay_w1(blocks, 8)
    wzp = np.zeros((D, 64), np.float32); wzp[:, 0:16] = wzf; wzp[:, 32:48] = wzb
    wz = np.ascontiguousarray(wzp.reshape(KC, 128, 64).transpose(1, 0, 2)).reshape(128, KC * 64)
    wvr = np.ascontiguousarray(np.concatenate([wv, wr], 1).reshape(KC, 128, 1024).transpose(1, 0, 2)).reshape(128, KC * 1024)
    wgate = np.ascontiguousarray(np.concatenate([wgf, wgb], 1)).astype(np.float32)
    bgate = np.ascontiguousarray(np.broadcast_to(np.concatenate([bgf, bgb])[None, :], (128, 512))).astype(np.float32)
    rope, tri, idf = gla_consts()
    return {"hTt": hTt, "wfm": wfm, "wz": wz, "wvr": wvr, "wgate": wgate, "bgate": bgate, "rope": rope,
            "ng": np.ascontiguousarray(np.broadcast_to(ngv[None, :], (128, 512))).astype(np.float32), "tri": tri, "identf": idf}


def kernel(x, c, ctx, c_ctx, ada_w, ada_b, norm_mix_g, norm_ffn_g, gla_w_in, gla_wg_fwd, gla_bg_fwd, gla_wg_bwd, gla_bg_bwd,
           gla_norm_g, gla_w_out, nat_w_in, nat_rpb, nat_w_out, ffn_w_gate, ffn_w_up, ffn_w_down, moe_router, moe_w_gate,
           moe_w_up, moe_w_down, final_norm_g, _probe=None):
    A = lambda a: np.asarray(a, np.float32)
    x, c, ctx, c_ctx = A(x), A(c), A(ctx), A(c_ctx)
    probe = _probe or (lambda name, val: None)
    identb = np.eye(128, dtype=np.float32).astype(ml_dtypes.bfloat16)
    ident8 = np.eye(8, dtype=np.float32)
    zeros = np.zeros(D, np.float32)
    res = _run(_prog("ada", build_ada), [lay_ada(A(ada_w), A(ada_b), c, c_ctx, k) for k in range(NCORES)])
    M = np.concatenate([r["m"].reshape(NR, NL, NCOL) for r in res], 2)
    probe("ada", M)

    def mods(layer, core):
        b = core // 2
        rows = (4 if core % 2 == 0 else b, b)
        return [[M[r, layer, j * D:(j + 1) * D] for r in rows] for j in range(6)]

    X = np.concatenate([np.concatenate([ctx[b], x[b]], 0) for b in range(4)], 0)
    xT = [np.ascontiguousarray(X[1152 * k:1152 * (k + 1)].T) for k in range(NCORES)]
    ims = []
    for k in range(NCORES):
        sh1, sc1, g1, sh2, sc2, g2 = mods(0, k)
        ims.append({"xT": xT[k], "vec": lay_vec([A(norm_mix_g)[0], sc1[0], sc1[1], sh1[0], sh1[1]])})
    res = _run(_prog("T0", lambda: build_tok("T0")), ims)
    HT = np.concatenate([r["hT"] for r in res], 1)
    probe("h_0", HT)
    for i in range(4):
        j = i // 2
        last = i == 3
        if i % 2 == 0:
            w_in = A(gla_w_in)[j]
            q_, k_, v_, r_ = w_in[:, 0:1024], w_in[:, 1024:2048], w_in[:, 2048:4096], w_in[:, 4096:6144]
            zf_, zb_ = w_in[:, 6144:6160], w_in[:, 6160:6176]
            ims = []
            for k in range(NCORES):
                hd, bg = k % 4, k // 4
                ims.append(gla_inputs(np.ascontiguousarray(HT[:, 4608 * bg:4608 * (bg + 1)]),
                                      q_[:, 256 * hd:256 * (hd + 1)], k_[:, 256 * hd:256 * (hd + 1)], v_[:, 512 * hd:512 * (hd + 1)],
                                      r_[:, 512 * hd:512 * (hd + 1)], zf_, zb_,
                                      A(gla_wg_fwd)[j][:, 256 * hd:256 * (hd + 1)], A(gla_wg_bwd)[j][:, 256 * hd:256 * (hd + 1)],
                                      A(gla_bg_fwd)[j][256 * hd:256 * (hd + 1)], A(gla_bg_bwd)[j][256 * hd:256 * (hd + 1)], A(gla_norm_g)[j]))
            res = _run(_prog("gla", lambda: build_gla(2)), ims)
            O = np.empty((NTOK, D), ml_dtypes.bfloat16)
            for k in range(NCORES):
                hd, bg = k % 4, k // 4
                O[4608 * bg:4608 * (bg + 1), 512 * hd:512 * (hd + 1)] = res[k]["Og"]
            w_out = A(gla_w_out)[j]
        else:
            w_in = A(nat_w_in)[j]
            ims = []
            for k in range(NCORES):
                cs = slice(256 * k, 256 * (k + 1))
                wqk = np.concatenate([lay_w1(w_in[:, 0:2048][:, cs], 2), lay_w1(w_in[:, 2048:4096][:, cs], 2)], 0)
                wv = np.ascontiguousarray(w_in[:, 4096:6144][:, cs].reshape(KC, 128, 256).transpose(1, 0, 2)).reshape(128, KC * 256)
                ims.append({"hT": HT, "wqk": wqk, "wv": wv, "bias": nat_bias_tiles(A(nat_rpb)[j][2 * k:2 * k + 2]), "identb": identb})
            res = _run(_prog("nat", lambda: build_nat(4)), ims)
            O = np.concatenate([r["O"] for r in res], 1)
            w_out = A(nat_w_out)[j]
        probe("O_%d" % i, O)
        moe = i % 2 == 1
        wo_l = lay_w1(w_out, KC)
        ims = []
        for k in range(NCORES):
            sh1, sc1, g1, sh2, sc2, g2 = mods(i, k)
            im = {"xT": xT[k], "OT": np.ascontiguousarray(O[1152 * k:1152 * (k + 1)].T), "wo": wo_l,
                  "vec": lay_vec([g1[0], g1[1], A(norm_ffn_g)[i], sc2[0], sc2[1], sh2[0], sh2[1]])}
            if moe:
                rt = A(moe_router)[j]
                im["rt"] = lay_vec([rt[:, e] for e in range(8)]); im["ident8"] = ident8
            ims.append(im)
        res = _run(_prog("Ta%d" % moe, lambda: build_tok("Ta", moe=moe)), ims)
        xT = [r["xo"] for r in res]
        H2T = np.concatenate([r["hT"] for r in res], 1)
        probe("xa_%d" % i, xT); probe("h2_%d" % i, H2T)
        ims = []
        if moe:
            G = np.concatenate([r["gates"] for r in res], 0)
            probe("gates_%d" % i, G)
            for k in range(NCORES):
                ims.append({"hT": H2T, "wg": lay_w1(A(moe_w_gate)[j][k], 44), "wu": lay_w1(A(moe_w_up)[j][k], 44),
                            "wd": lay_w2(A(moe_w_down)[j][k], 44), "gate": np.ascontiguousarray(G[:, k]).reshape(1, NTOK)})
            res = _run(_prog("ffn44", lambda: build_ffn(NTOK, 44)), ims)
        else:
            ones = np.ones((1, NTOK), np.float32)
            for k in range(NCORES):
                fs = slice(704 * k, 704 * (k + 1))
                ims.append({"hT": H2T, "wg": lay_w1(A(ffn_w_gate)[j][:, fs], 6), "wu": lay_w1(A(ffn_w_up)[j][:, fs], 6),
                            "wd": lay_w2(A(ffn_w_down)[j][fs, :], 6), "gate": ones})
            res = _run(_prog("ffn6", lambda: build_ffn(NTOK, 6)), ims)
        YP = [r["yT"] for r in res]
        probe("yp_%d" % i, YP)
        ims = []
        for k in range(NCORES):
            sh1, sc1, g1, sh2, sc2, g2 = mods(i, k)
            if last:
                cols = [g2[0], g2[1], A(final_norm_g), zeros, zeros, zeros, zeros]
            else:
                nsh1, nsc1 = mods(i + 1, k)[0], mods(i + 1, k)[1]
                cols = [g2[0], g2[1], A(norm_mix_g)[i + 1], nsc1[0], nsc1[1], nsh1[0], nsh1[1]]
            yp = np.stack([np.ascontiguousarray(YP[e][:, 1152 * k:1152 * (k + 1)]) for e in range(NCORES)], 0)
            ims.append({"xT": xT[k], "yP": yp, "identb": identb, "vec": lay_vec(cols)})
        res = _run(_prog("Tb%d" % last, lambda: build_tok("Tb", final=last)), ims)
        xT = [r["xo"] for r in res]
        HT = np.concatenate([r["hT"] for r in res], 1)
        probe("xb_%d" % i, xT)
        if not last:
            probe("h_%d" % (i + 1), HT)
    out_all = np.ascontiguousarray(HT.T).astype(np.float32)
    return np.stack([out_all[2304 * b + 256:2304 * (b + 1)] for b in range(4)], 0)
```
